# Optimizing a Trainium2 kernel written in Bass

```python
import jax, jax.numpy as jnp
from jax import lax
import numpy as np

D_MODEL = 1024
BATCH = 16
SEQ = 2048
DEPTH = 2

D_MIX = D_MODEL
HEAD_DIM = 64
CONV_WIDTH = 256
SB_HEADS = 6
SB_WIDTH = SB_HEADS * HEAD_DIM
RWKV_HEADS = 6
RWKV_WIDTH = RWKV_HEADS * HEAD_DIM
CONV_TAPS = 31
DECAY_LORA = 32
ICLR_LORA = 32
VRES_LORA = 32
BLOCK_Q = 128
RMS_EPS = 1e-6
LN_EPS = 1e-5
GN_EPS = 64e-5
IN_WIDTHS = (CONV_WIDTH, CONV_WIDTH, CONV_WIDTH,
             SB_WIDTH, SB_WIDTH, SB_WIDTH, SB_WIDTH,
             RWKV_WIDTH, RWKV_WIDTH, RWKV_WIDTH,
             DECAY_LORA, ICLR_LORA,
             RWKV_WIDTH)
D_IN = 3 * CONV_WIDTH + 4 * SB_WIDTH + 4 * RWKV_WIDTH + DECAY_LORA + ICLR_LORA
SHIFT_WIDTH = 3 * RWKV_WIDTH + DECAY_LORA + ICLR_LORA

kernel_name = 'hybrid_conformer_stickbreak_rwkv7'


def _split_cols(p, widths):
    idx, acc = [], 0
    for w in widths[:-1]:
        acc += w
        idx.append(acc)
    return jnp.split(p, idx, axis=-1)


def rms_norm(x, g):
    xf = x.astype(jnp.float32)
    y = xf * lax.rsqrt(jnp.mean(xf * xf, axis=-1, keepdims=True) + RMS_EPS)
    return (y * g.astype(jnp.float32)).astype(x.dtype)


def token_shift(f):
    return jnp.pad(f, ((0, 0), (1, 0), (0, 0)))[:, :-1]


def _heads(t, n_heads):
    b, s, _ = t.shape
    return t.reshape(b, s, n_heads, HEAD_DIM).transpose(0, 2, 1, 3)


def conformer_conv(glu_a, glu_b, dw, dw_b, ln_g, ln_b, pw, pw_b):
    u = glu_a * jax.nn.sigmoid(glu_b)
    u = lax.conv_general_dilated(u, dw[:, None, :].astype(u.dtype), window_strides=(1,),
                                 padding=((CONV_TAPS - 1, 0),),
                                 dimension_numbers=('NWC', 'WIO', 'NWC'),
                                 feature_group_count=CONV_WIDTH) + dw_b
    uf = u.astype(jnp.float32)
    mean = jnp.mean(uf, axis=-1, keepdims=True)
    var = jnp.mean(jnp.square(uf - mean), axis=-1, keepdims=True)
    uf = (uf - mean) * lax.rsqrt(var + LN_EPS) * ln_g + ln_b
    uf = jax.nn.silu(uf)
    return uf.astype(glu_a.dtype) @ pw + pw_b


def stick_breaking_attention(q, k, v):
    b, h, t_len, dh = q.shape
    scale = HEAD_DIM ** -0.5
    outs = []
    for blk in range(t_len // BLOCK_Q):
        q0, q1 = blk * BLOCK_Q, (blk + 1) * BLOCK_Q
        z = jnp.einsum('bhqd,bhkd->bhqk', q[:, :, q0:q1], k[:, :, :q1]).astype(jnp.float32) * scale
        causal = jnp.arange(q1)[None, :] < jnp.arange(q0, q1)[:, None]
        log_keep = jnp.where(causal, jax.nn.log_sigmoid(-z), 0.0)
        log_rest = lax.cumsum(log_keep, axis=3, reverse=True) - log_keep
        att = jnp.where(causal, jnp.exp(jax.nn.log_sigmoid(z) + log_rest), 0.0)
        outs.append(jnp.einsum('bhqk,bhkd->bhqd', att.astype(v.dtype), v[:, :, :q1]))
    o = jnp.concatenate(outs, axis=2)
    return o.transpose(0, 2, 1, 3).reshape(b, t_len, h * dh)


def rwkv7_scan(r, w, k, v, a, b):
    bsz, _, h, n = r.shape

    def step(S, inp):
        r_t, w_t, k_t, v_t, a_t, b_t = inp
        sa = jnp.einsum('bhvk,bhk->bhv', S, a_t)
        S = S * w_t[:, :, None, :] + sa[..., None] * b_t[:, :, None, :] + v_t[..., None] * k_t[:, :, None, :]
        return S, jnp.einsum('bhvk,bhk->bhv', S, r_t)

    S0 = jnp.zeros((bsz, h, n, n), jnp.float32)
    xs = tuple(t.transpose(1, 0, 2, 3) for t in (r, w, k, v, a, b))
    _, y = lax.scan(step, S0, xs)
    return y.transpose(1, 0, 2, 3)


def rwkv7_time_mix(r, k, v, w_d, a_d, mu, w0, w2, a0, a2, kk_scale, ka, rk, gn_g, gn_b, v_first, v_res):
    out_dtype = r.dtype
    feats = jnp.concatenate([r, k, v, w_d, a_d], axis=-1)
    feats = feats + (token_shift(feats) - feats) * mu
    r, k, v, w_d, a_d = _split_cols(feats.astype(jnp.float32), (RWKV_WIDTH, RWKV_WIDTH, RWKV_WIDTH, DECAY_LORA, ICLR_LORA))
    w_log = -jax.nn.softplus(-(w0 + jnp.tanh(w_d) @ w2)) - 0.5
    decay = jnp.exp(-jnp.exp(w_log))
    a = jax.nn.sigmoid(a0 + a_d @ a2)
    if v_res is None:
        v_first = v
    else:
        v0, v1, v2 = v_res
        v = v + (v_first - v) * jax.nn.sigmoid(v0 + (v @ v1) @ v2)
    bsz, t_len, _ = r.shape
    hd = lambda t: t.reshape(bsz, t_len, RWKV_HEADS, HEAD_DIM)
    kk = hd(k * kk_scale)
    kk = kk / jnp.maximum(jnp.sqrt(jnp.sum(kk * kk, axis=-1, keepdims=True)), 1e-12)
    k = k * (1.0 + (a - 1.0) * ka)
    rh, kh, vh, ah = hd(r), hd(k), hd(v), hd(a)
    y = rwkv7_scan(rh, hd(decay), kh, vh, -kk, kk * ah)
    mean = jnp.mean(y, axis=-1, keepdims=True)
    var = jnp.mean(jnp.square(y - mean), axis=-1, keepdims=True)
    y = ((y - mean) * lax.rsqrt(var + GN_EPS)).reshape(bsz, t_len, RWKV_WIDTH) * gn_g + gn_b
    y = y + (jnp.sum(rh * kh * rk, axis=-1, keepdims=True) * vh).reshape(bsz, t_len, RWKV_WIDTH)
    return y.astype(out_dtype), v_first


def setup_inputs(seed: int = 0) -> dict:
    key = jax.random.key(seed)
    ks = jax.random.split(key, 24)
    L = DEPTH
    nrm = lambda k, shape, s: jax.random.normal(k, shape, jnp.float32) * s
    return {
        'x': nrm(ks[0], (BATCH, SEQ, D_MODEL), 1.0),
        'pre_norm_g': 1.0 + nrm(ks[1], (L, D_MODEL), 0.02),
        'post_norm_g': 1.0 + nrm(ks[2], (L, D_MODEL), 0.02),
        'w_in': nrm(ks[3], (L, D_MODEL, D_IN), D_MODEL ** -0.5),
        'w_out': nrm(ks[4], (L, D_MIX, D_MODEL), D_MIX ** -0.5),
        'conv_dw': nrm(ks[5], (L, CONV_TAPS, CONV_WIDTH), CONV_TAPS ** -0.5),
        'conv_dw_b': nrm(ks[6], (L, CONV_WIDTH), 0.02),
        'conv_ln_g': 1.0 + nrm(ks[7], (L, CONV_WIDTH), 0.02),
        'conv_ln_b': nrm(ks[8], (L, CONV_WIDTH), 0.02),
        'conv_pw': nrm(ks[9], (L, CONV_WIDTH, CONV_WIDTH), CONV_WIDTH ** -0.5),
        'conv_pw_b': nrm(ks[10], (L, CONV_WIDTH), 0.02),
        'rwkv_mu': jax.random.uniform(ks[11], (L, SHIFT_WIDTH), jnp.float32),
        'rwkv_w0': jax.random.uniform(ks[12], (L, RWKV_WIDTH), jnp.float32, minval=-4.0, maxval=0.0),
        'rwkv_w2': nrm(ks[13], (L, DECAY_LORA, RWKV_WIDTH), 0.1),
        'rwkv_a0': nrm(ks[14], (L, RWKV_WIDTH), 0.1),
        'rwkv_a2': nrm(ks[15], (L, ICLR_LORA, RWKV_WIDTH), 0.1),
        'rwkv_kk_scale': 0.85 + nrm(ks[16], (L, RWKV_WIDTH), 0.05),
        'rwkv_ka': 1.0 + nrm(ks[17], (L, RWKV_WIDTH), 0.05),
        'rwkv_rk': nrm(ks[18], (L, RWKV_HEADS, HEAD_DIM), 0.1),
        'rwkv_gn_g': 1.0 + nrm(ks[19], (L, RWKV_WIDTH), 0.02),
        'rwkv_gn_b': nrm(ks[20], (L, RWKV_WIDTH), 0.02),
        'rwkv_v0': nrm(ks[21], (L - 1, RWKV_WIDTH), 0.1),
        'rwkv_v1': nrm(ks[22], (L - 1, RWKV_WIDTH, VRES_LORA), RWKV_WIDTH ** -0.5),
        'rwkv_v2': nrm(ks[23], (L - 1, VRES_LORA, RWKV_WIDTH), 0.1),
    }


def reference(x, pre_norm_g, post_norm_g, w_in, w_out, conv_dw, conv_dw_b, conv_ln_g, conv_ln_b,
              conv_pw, conv_pw_b, rwkv_mu, rwkv_w0, rwkv_w2, rwkv_a0, rwkv_a2, rwkv_kk_scale, rwkv_ka,
              rwkv_rk, rwkv_gn_g, rwkv_gn_b, rwkv_v0, rwkv_v1, rwkv_v2):
    v_first = None
    for l in range(DEPTH):
        h = rms_norm(x, pre_norm_g[l])
        p = h @ w_in[l]
        (glu_a, glu_b, g_conv, q, k, v, g_sb, r_r, r_k, r_v, w_d, a_d, g_rwkv) = _split_cols(p, IN_WIDTHS)
        y_conv = conformer_conv(glu_a, glu_b, conv_dw[l], conv_dw_b[l], conv_ln_g[l], conv_ln_b[l],
                                conv_pw[l], conv_pw_b[l])
        y_sb = stick_breaking_attention(_heads(q, SB_HEADS), _heads(k, SB_HEADS), _heads(v, SB_HEADS))
        v_res = None if l == 0 else (rwkv_v0[l - 1], rwkv_v1[l - 1], rwkv_v2[l - 1])
        y_rwkv, v_first = rwkv7_time_mix(r_r, r_k, r_v, w_d, a_d, rwkv_mu[l], rwkv_w0[l], rwkv_w2[l],
                                         rwkv_a0[l], rwkv_a2[l], rwkv_kk_scale[l], rwkv_ka[l], rwkv_rk[l],
                                         rwkv_gn_g[l], rwkv_gn_b[l], v_first, v_res)
        mix = jnp.concatenate([y_conv * jax.nn.silu(g_conv),
                               y_sb * jax.nn.silu(g_sb),
                               y_rwkv * jax.nn.silu(g_rwkv)], axis=-1)
        x = x + rms_norm(mix @ w_out[l], post_norm_g[l])
    return x
```

```python
import contextlib
import numpy as np
import concourse.bass as bass
import concourse.mybir as mybir
from concourse.bass_utils import run_bass_kernel_spmd

F32 = mybir.dt.float32
BF16 = mybir.dt.bfloat16
AF = mybir.ActivationFunctionType
ALU = mybir.AluOpType
AX = mybir.AxisListType

ENGS = ['pe', 'act', 'dve', 'pool', 'sp']
NDMASEM = 24
EPOCH = 30000

D = 1024
KC = 8
TB = 512
HD = 64
CONVW = 256
TAPS = 31
D_IN = 3904
C_GA, C_GB, C_GC, C_Q, C_K, C_V, C_GS = 0, 256, 512, 768, 1152, 1536, 1920
C_RR, C_RK, C_RV, C_WD, C_AD, C_GR = 2304, 2688, 3072, 3456, 3488, 3520
RMS_EPS = 1e-6
LN_EPS = 1e-5
GN_EPS = 64e-5
P_PRE, P_POST, P_DW, P_DWB, P_LNG, P_LNB, P_PWB = 0, 8, 16, 78, 80, 82, 84
P_MU, P_W0, P_A0, P_KKS, P_KA, P_V0, P_MUWA, NPP = 86, 95, 98, 101, 104, 107, 110, 112
W_PW, W_W2A2, W_V1, W_V2, W_RKM, NWS = 0, 512, 896, 992, 1376, 1384


def _box(ap):
    t = ap.tensor
    name = t.name
    pat = [list(x) for x in ap.ap]
    off = int(ap.offset)
    tn = type(t).__name__
    if 'PSum' in tn:
        return (name, 0, 128, 0, 1 << 30)
    if 'DRam' in tn:
        ext = 1
        for s, c in pat:
            ext += (c - 1) * abs(s)
        return (name, 0, 1, off, off + ext)
    F = 1
    for s in list(t.shape)[1:]:
        F *= s
    p0 = off // F
    f0 = off % F
    pstep, pcnt = pat[0]
    pc = 1 if pstep == 0 else (pcnt - 1) * (pstep // F) + 1
    ext = 1
    for s, c in pat[1:]:
        ext += (c - 1) * abs(s)
    return (name, p0, p0 + pc, f0, f0 + ext)


def _ovl(a, b):
    return a[1] < b[2] and b[1] < a[2] and a[3] < b[4] and b[3] < a[4]


def _contains(a, b):
    return a[1] <= b[1] and b[2] <= a[2] and a[3] <= b[3] and b[4] <= a[4]


class Sched:
    def __init__(self, nc):
        self.nc = nc
        self.items = {e: [] for e in ENGS}
        self.count = {e: 0 for e in ENGS}
        self.known = {e: {f: 0 for f in ENGS} for e in ENGS}
        self.opknown = {e: [None] for e in ENGS}
        self.dma_known = {e: set() for e in ENGS}
        self.acc = {}
        self.ndma = 0
        self.dma_info = []

    def _deps(self, reads, writes):
        deps = []
        rb = [_box(a) for a in reads]
        wb = [_box(a) for a in writes]
        for b in rb:
            for rec in self.acc.get(b[0], ()):
                if rec[3] and _ovl(rec[0], b):
                    deps.append(rec)
        for b in wb:
            for rec in self.acc.get(b[0], ()):
                if _ovl(rec[0], b):
                    deps.append(rec)
        return deps, rb, wb

    def _record(self, eng, idx, dma_id, rb, wb):
        for b in wb:
            lst = self.acc.setdefault(b[0], [])
            lst[:] = [r for r in lst if not _contains(b, r[0])]
            lst.append((b, eng, idx, True, dma_id))
        for b in rb:
            lst = self.acc.setdefault(b[0], [])
            if dma_id is None:
                lst[:] = [r for r in lst if not (r[0] == b and r[1] == eng and not r[3] and r[4] is None)]
            lst.append((b, eng, idx, False, dma_id))

    def _emit_waits(self, eng, deps):
        kn = self.known[eng]
        for (b, e2, idx, isw, dma_id) in deps:
            if dma_id is not None:
                if dma_id in self.dma_known[eng]:
                    continue
                slot, val = self.dma_info[dma_id]
                self.items[eng].append(('wait', ('dma', slot), val))
                self.dma_known[eng].add(dma_id)
                continue
            if e2 == eng and eng == 'pe':
                continue
            if kn[e2] >= idx:
                continue
            self.items[eng].append(('wait', ('eng', e2, (idx - 1) // EPOCH), ((idx - 1) % EPOCH) + 1))
            kn[e2] = idx
            ok = self.opknown[e2][idx]
            for f in ENGS:
                if f != eng and ok[f] > kn[f]:
                    kn[f] = ok[f]

    def op(self, eng, fn, reads=(), writes=()):
        deps, rb, wb = self._deps(reads, writes)
        self._emit_waits(eng, deps)
        self.count[eng] += 1
        idx = self.count[eng]
        self.opknown[eng].append(dict(self.known[eng]))
        self.items[eng].append(('op', fn, ('eng', eng, (idx - 1) // EPOCH)))
        self._record(eng, idx, None, rb, wb)

    def dma(self, eng, out, in_, **kw):
        deps, rb, wb = self._deps([in_], [out])
        self._emit_waits(eng, deps)
        i = self.ndma
        self.ndma += 1
        slot = i % NDMASEM
        val = 16 * (i // NDMASEM + 1)
        if i >= NDMASEM and (i - NDMASEM) not in self.dma_known[eng]:
            self.items[eng].append(('wait', ('dma', slot), val - 16))
            self.dma_known[eng].add(i - NDMASEM)
        self.dma_info.append((slot, val))
        self.items[eng].append(('dma', (lambda e, o=out, n=in_, k=kw: e.dma_start(out=o, in_=n, **k)), ('dma', slot)))
        self._record(eng, None, i, rb, wb)
        return i

    def wait_all_dma(self, eng):
        last = {}
        for i, (slot, val) in enumerate(self.dma_info):
            last[slot] = val
        for slot, val in last.items():
            self.items[eng].append(('wait', ('dma', slot), val))

    def emit(self):
        nc = self.nc
        with contextlib.ExitStack() as st:
            sems = {}
            for e in ENGS:
                nep = (self.count[e] + EPOCH - 1) // EPOCH
                for k in range(max(nep, 1)):
                    sems[('eng', e, k)] = st.enter_context(nc.semaphore(f"s_{e}_{k}"))
            for s in range(NDMASEM):
                sems[('dma', s)] = st.enter_context(nc.semaphore(f"s_dma_{s}"))
            block = st.enter_context(nc.Block())
            items = self.items

            def run(engname, engobj):
                for it in items[engname]:
                    if it[0] == 'wait':
                        engobj.wait_ge(sems[it[1]], it[2])
                    elif it[0] == 'op':
                        it[1](engobj).then_inc(sems[it[2]], 1)
                    else:
                        it[1](engobj).then_inc(sems[it[2]], 16)

            @block.tensor
            def _(e):
                run('pe', e)

            @block.scalar
            def _(e):
                run('act', e)

            @block.vector
            def _(e):
                run('dve', e)

            @block.gpsimd
            def _(e):
                run('pool', e)

            @block.sync
            def _(e):
                run('sp', e)


def build(T=2048, NSEQ=2, NL=2, dbg=None, phases=('conv', 'attn', 'rwkv')):
    NB = T // TB
    nc = bass.Bass("TRN2", target_bir_lowering=False)
    dt = nc.dram_tensor
    xT = dt("xT", [NSEQ, D, T], F32, kind="ExternalInput").ap()
    yT = dt("yT", [NSEQ, D, T], F32, kind="ExternalOutput").ap()
    w_in = dt("w_in", [NL, D, D_IN], F32, kind="ExternalInput").ap()
    w_out = dt("w_out", [NL, D, D], F32, kind="ExternalInput").ap()
    ppd = dt("pp", [NL, 128, NPP], F32, kind="ExternalInput").ap()
    wsmd = dt("wsm", [NL, 128, NWS], F32, kind="ExternalInput").ap()
    fbd = dt("fb", [NL, 768], F32, kind="ExternalInput").ap()
    x1 = [[dt(f"x1_{s}_{b}", [D, TB], F32).ap() for b in range(NB)] for s in range(NSEQ)]
    vfd = [[dt(f"vf_{s}_{b}", [384, TB], F32).ap() for b in range(NB)] for s in range(NSEQ)]
    wob_d = [dt(f"wobd{l}", [KC, 128, KC, 128], BF16).ap() for l in range(NL)]
    if dbg is not None:
        dbgd = dt("dbg", [D, T], BF16, kind="ExternalOutput").ap()

    with contextlib.ExitStack() as st:
        def sb(n, s, d):
            return st.enter_context(nc.sbuf_tensor(n, s, d))
        S = Sched(nc)

        win = sb("win", [128, KC, D_IN], BF16)
        wob = [sb(f"wob{i}", [128, KC, 128], BF16) for i in range(2)]
        wsm = sb("wsm_bf", [128, NWS], BF16)
        pp = sb("pp_sb", [128, NPP], F32)
        pq = sb("pq_sb", [128, NPP - P_MU], F32)
        fb = sb("fb_sb", [128, 768], F32)
        ident = sb("ident", [128, 128], BF16)
        ident32 = sb("ident32", [128, 128], F32)
        negmask = sb("negmask", [128, 128], BF16)
        negU = sb("negU", [128, 128], BF16)
        negL = sb("negL", [128, 128], BF16)
        mX = sb("mX", [128, 384], F32)
        onesD = sb("onesD", [128, 128], BF16)
        ones256 = sb("ones256", [128, 128], BF16)
        bdones = sb("bdones", [128, 128], BF16)
        rmask = sb("rmask", [128, TB], F32)
        E64 = sb("E64", [128, 64], F32)
        kTc = sb("kTc", [128, 3, T], BF16)
        vc = sb("vc", [128, T // 128, 384], BF16)
        hT = sb("hT", [128, KC, TB], BF16)
        mixT = sb("mixT", [128, KC, TB], BF16)
        ubuf = sb("ubuf", [128, 2, TB + 30], BF16)
        hist = sb("hist", [128, 16], F32)
        Hs = sb("Hs", [128, 3, 64], BF16)
        NT32 = 11
        t32 = [sb(f"t32_{i}", [128, TB], F32) for i in range(NT32)]
        NTBF = 7
        tbf = [sb(f"tbf_{i}", [128, TB], BF16) for i in range(NTBF)]
        rawb = sb("rawb", [128, TB + 1], F32)
        VP = sb("VP", [128, 3, TB], F32)
        ARp = [sb(f"AR{i}", [128, 4, 2, 128], BF16) for i in range(2)]
        TOKp = [sb(f"TOK{i}", [128, 4, 3, 128], BF16) for i in range(2)]
        BT1 = sb("BT1", [128, TB], BF16)
        KT1 = sb("KT1", [128, TB], BF16)
        y_ys = sb("y_ys", [128, TB], F32)
        y_sq = sb("y_sq", [128, TB], F32)
        y_dd = sb("y_dd", [128, TB], F32)
        CM = sb("CM", [128, 8, 3, 128], BF16)
        QQ = sb("QQ", [128, 8, 2, 128], BF16)
        Zt = sb("Zt", [128, 8, 128], BF16)
        dGtp = [sb(f"dGt{i}", [128, 4, 64], BF16) for i in range(2)]
        sgrp = [sb(f"sgr{i}", [128, 4, 128], BF16) for i in range(2)]
        sm = sb("sm", [128, 64], F32)
        XU = sb("XU", [128, 2, 128], BF16)
        a_sgs = sb("a_sgs", [128, TB], BF16)
        a_qT = sb("a_qT", [128, TB], BF16)
        a_e = [sb(f"a_e{i}", [128, TB], F32) for i in range(3)]
        a_sp = [sb(f"a_sp{i}", [128, TB], BF16) for i in range(3)]
        a_ec = [sb(f"a_ec{i}", [128, TB], F32) for i in range(2)]
        a_att = [sb(f"a_att{i}", [128, TB], BF16) for i in range(2)]
        P = [st.enter_context(nc.psum_tensor(f"P{i}", [128, 512], F32)) for i in range(7)]
        PT = st.enter_context(nc.psum_tensor("PT", [128, 1024], BF16))
        bank_i = [0]
        bpool = [P[0:7]]
        rec = [None]

        def nb():
            bl = bpool[0]
            b = bl[bank_i[0] % len(bl)]
            bank_i[0] += 1
            return b

        recl = []

        def _op(eng, fn, reads=(), writes=()):
            if rec[0] is not None:
                recl.append((rec[0], ('op', eng, fn, list(reads), list(writes))))
            else:
                S.op(eng, fn, reads, writes)

        def _dma(eng, out, in_):
            if rec[0] is not None:
                recl.append((rec[0], ('dma', eng, out, in_)))
            else:
                S.dma(eng, out, in_)

        def _fsize(ap):
            n = 1
            for st_, cnt in list(ap.ap)[1:]:
                n *= cnt
            return n

        def flush_sched():
            n = len(recl)
            accd = {}
            deps_all, durs, engs, sids = [], [], [], []
            for idx, (sid, it) in enumerate(recl):
                sids.append(sid)
                if it[0] == 'op':
                    eng, reads, writes = it[1], it[3], it[4]
                else:
                    eng, reads, writes = 'sp', [it[3]], [it[2]]
                rb = [_box(a_) for a_ in reads]
                wb = [_box(a_) for a_ in writes]
                dp = set()
                for bx_ in rb:
                    for r in accd.get(bx_[0], ()):
                        if r[2] and _ovl(r[0], bx_):
                            dp.add(r[1])
                for bx_ in wb:
                    for r in accd.get(bx_[0], ()):
                        if _ovl(r[0], bx_):
                            dp.add(r[1])
                for bx_ in wb:
                    l_ = accd.setdefault(bx_[0], [])
                    l_[:] = [r for r in l_ if not _contains(bx_, r[0])]
                    l_.append((bx_, idx, True))
                for bx_ in rb:
                    l_ = accd.setdefault(bx_[0], [])
                    l_[:] = [r for r in l_ if not (r[0] == bx_ and not r[2] and sids[r[1]] == sid)]
                    l_.append((bx_, idx, False))
                nel = _fsize(writes[0]) if writes else 64
                if eng == 'pe':
                    d_ = max(nel, 64) / 1400.0
                elif eng == 'act':
                    d_ = nel / 1200.0 + 0.2
                elif eng == 'dve':
                    d_ = nel / 960.0 + 0.12
                elif eng == 'pool':
                    d_ = nel / 450.0 + 0.15
                else:
                    d_ = 2.5
                deps_all.append(dp)
                durs.append(d_)
                engs.append(eng)
            nstream = (max(sids) + 1) if sids else 0
            queues = [[i for i in range(n) if sids[i] == k] for k in range(nstream)]
            ptr = [0] * nstream
            fin = [0.0] * n
            done = [False] * n
            efree = {e: 0.0 for e in ENGS}
            left = n
            filler_run = [0]
            while left:
                best = None
                fallback = None
                for k in range(nstream):
                    if ptr[k] >= len(queues[k]):
                        continue
                    i = queues[k][ptr[k]]
                    ok = True
                    ready = 0.0
                    for j in deps_all[i]:
                        if not done[j]:
                            ok = False
                            break
                        t_ = fin[j] + (0.15 if engs[j] == engs[i] else 0.8)
                        if t_ > ready:
                            ready = t_
                    if not ok:
                        continue
                    start = max(ready, efree[engs[i]])
                    if k == 3 and engs[i] == 'dve' and filler_run[0] >= 2:
                        if fallback is None:
                            fallback = (start, k, i)
                        continue
                    if best is None or start < best[0]:
                        best = (start, k, i)
                if best is None:
                    best = fallback
                start, k, i = best
                if engs[i] == 'dve':
                    filler_run[0] = filler_run[0] + 1 if k == 3 else 0
                fin[i] = start + durs[i]
                efree[engs[i]] = fin[i]
                done[i] = True
                ptr[k] += 1
                left -= 1
                it = recl[i][1]
                if it[0] == 'op':
                    S.op(it[1], it[2], it[3], it[4])
                else:
                    S.dma(it[1], it[2], it[3])
            del recl[:]

        def mm(out, lhsT, rhs, start=True, stop=True):
            _op('pe', lambda e: e.matmul(out, lhsT, rhs, start=start, stop=stop), [lhsT, rhs], [out])

        def tr(out, in_, idn):
            _op('pe', lambda e: e.transpose(out, in_, idn), [in_, idn], [out])

        def act(out, in_, func, bias=None, scale=1.0):
            rd = [in_]
            kw = {}
            if bias is not None:
                kw['bias'] = bias
                if not isinstance(bias, float):
                    rd.append(bias)
            _op('act', lambda e: e.activation(out, in_, func, scale=scale, **kw), rd, [out])

        def tt(eng, out, a, b, op):
            _op(eng, lambda e: e.tensor_tensor(out, a, b, op), [a, b], [out])

        def ts(eng, out, a, s1, s2, op0, op1=None):
            rd = [a] + [s for s in (s1, s2) if s is not None and not isinstance(s, float)]
            if op1 is None and eng == 'pool' and op0 == ALU.mult:
                _op(eng, lambda e: e.tensor_scalar(out, a, s1, 0.0, ALU.mult, ALU.add), rd, [out])
            elif op1 is None:
                _op(eng, lambda e: e.tensor_scalar(out, a, s1, None, op0), rd, [out])
            else:
                _op(eng, lambda e: e.tensor_scalar(out, a, s1, s2, op0, op1), rd, [out])

        def stt(out, a, s, b, op0, op1):
            rd = [a, b] + ([] if isinstance(s, float) else [s])
            _op('dve', lambda e: e.scalar_tensor_tensor(out, a, s, b, op0, op1), rd, [out])

        def cp(eng, out, in_):
            if eng == 'act':
                _op('act', lambda e: e.copy(out, in_), [in_], [out])
            else:
                _op(eng, lambda e: e.tensor_copy(out, in_), [in_], [out])

        ew_i = [0]

        def EW():
            ew_i[0] += 1
            return 'dve'

        def recip(out, in_):
            _op('dve', lambda e: e.reciprocal(out, in_), [in_], [out])

        def memset(eng, out, v):
            _op(eng, lambda e: e.memset(out, v), [], [out])

        def asel(out, in_, pattern, op, fill, base, cm):
            _op('pool', lambda e: e.affine_select(out, in_, pattern, op, fill, base=base, channel_multiplier=cm),
                 [in_], [out])

        memset('pool', ident32[:], 1.0)
        asel(ident32[:], ident32[:], [[-1, 128]], ALU.is_equal, 0.0, 0, 1)
        cp('pool', ident[:], ident32[:])
        memset('pool', t32[0][:, 0:128], -30000.0)
        asel(t32[0][:, 0:128], t32[0][:, 0:128], [[-1, 128]], ALU.is_ge, 0.0, 0, 1)
        cp('pool', negmask[:], t32[0][:, 0:128])
        memset('pool', t32[0][:, 0:128], -1.0)
        asel(t32[0][:, 0:128], t32[0][:, 0:128], [[-1, 128]], ALU.is_ge, 0.0, 0, 1)
        cp('pool', negU[:], t32[0][:, 0:128])
        memset('pool', t32[0][:, 0:128], -1.0)
        asel(t32[0][:, 0:128], t32[0][:, 0:128], [[1, 128]], ALU.is_gt, 0.0, 0, -1)
        cp('pool', negL[:], t32[0][:, 0:128])
        memset('pool', mX[:], 1.0)
        asel(mX[:, 0:128], mX[:, 0:128], [[-1, 128]], ALU.is_gt, 0.0, 0, 1)
        asel(mX[:, 128:256], mX[:, 128:256], [[1, 128]], ALU.is_gt, 0.0, 0, -1)
        asel(mX[:, 256:384], mX[:, 256:384], [[1, 128]], ALU.is_ge, 0.0, 0, -1)
        memset('pool', onesD[:], 1.0 / D)
        memset('pool', ones256[:], 1.0 / CONVW)
        memset('pool', bdones[:], 0.0)
        memset('pool', bdones[0:64, 0:64], 1.0)
        memset('pool', bdones[64:128, 64:128], 1.0)
        memset('pool', rmask[:], 1.0)
        for c in range(TB // 128):
            memset('pool', rmask[:, c * 128:c * 128 + 1], 0.0)
        memset('pool', E64[:], 0.0)
        cp('pool', E64[0:64, :], ident32[0:64, 0:64])
        cp('pool', E64[64:128, :], ident32[64:128, 64:128])

        def load_weights(l):
            _dma('sp', pp[:], ppd[l])
            ts('pool', pq[:], pp[:, P_MU:NPP], -1.0, 1.0, ALU.mult, ALU.add)
            _dma('sp', fb[:], fbd[l:l + 1, :].broadcast_to([128, 768]))
            k = 0
            CW = 488
            stage = [t32[5], t32[6], t32[7], t32[8], t32[9], t32[10]]
            ceng = ['pool', 'act', 'dve', 'act', 'dve', 'act']
            for c in range(KC):
                for q in range(D_IN // CW):
                    sg_ = stage[k % 6]
                    k += 1
                    _dma('sp', sg_[:, 0:CW], w_in[l, c * 128:(c + 1) * 128, q * CW:(q + 1) * CW])
                    cp(ceng[k % 6], win[:, c, q * CW:(q + 1) * CW], sg_[:, 0:CW])
            for c in range(KC):
                for q in range(2):
                    sg_ = stage[k % 6]
                    k += 1
                    _dma('sp', sg_[:, 0:512], w_out[l, c * 128:(c + 1) * 128, q * 512:(q + 1) * 512])
                    wb_ = tbf[k % 6]
                    cp(ceng[k % 6], wb_[:], sg_[:, 0:512])
                    _dma('sp', wob_d[l][q * 4:(q + 1) * 4, :, c, :].rearrange("d p n -> p d n"),
                         wb_[:].rearrange("p (d n) -> p d n", n=128))
            for q in range(4):
                sg_ = stage[k % 6]
                k += 1
                _dma('sp', sg_[:, 0:NWS // 4], wsmd[l, :, q * (NWS // 4):(q + 1) * (NWS // 4)])
                cp(ceng[k % 6], wsm[:, q * (NWS // 4):(q + 1) * (NWS // 4)], sg_[:, 0:NWS // 4])

        def proj_fm(ps, col0, M):
            for c in range(KC):
                mm(ps, win[:, c, col0:col0 + M], hT[:, c, :], start=(c == 0), stop=(c == KC - 1))

        def proj_tm(ps, ti, col0, N):
            for c in range(KC):
                mm(ps, hT[:, c, ti * 128:(ti + 1) * 128], win[:, c, col0:col0 + N], start=(c == 0), stop=(c == KC - 1))

        def conv_branch():
            ycv = [t32[1], t32[2]]
            tM, tV, tS = t32[3], t32[4], t32[5]
            bf_ = [tbf[1], tbf[4], tbf[5], tbf[6]]
            PTf = PT[:, :].bitcast(F32)
            nb = lambda: PTf
            for ch in range(2):
                pb_ = nb()
                proj_fm(pb_[:], C_GB + ch * 128, 128)
                act(tS[:], pb_[:], AF.Sigmoid)
                pa = nb()
                proj_fm(pa[:], C_GA + ch * 128, 128)
                tt('dve', ubuf[:, ch, 30:30 + TB], pa[:], tS[:], ALU.mult)
            for ch in range(2):
                ts('dve', ycv[ch][:], ubuf[:, ch, 0:TB], pp[:, P_DW + ch * TAPS:P_DW + ch * TAPS + 1],
                   pp[:, P_DWB + ch:P_DWB + ch + 1], ALU.mult, ALU.add)
            for j in range(1, TAPS):
                for ch in range(2):
                    stt(ycv[ch][:], ubuf[:, ch, j:j + TB], pp[:, P_DW + ch * TAPS + j:P_DW + ch * TAPS + j + 1],
                        ycv[ch][:], ALU.mult, ALU.add)
            for ch in range(2):
                cp('dve', ubuf[:, ch, 0:30], ubuf[:, ch, TB:TB + 30])
            pm = nb()
            for ch in range(2):
                cp('dve', bf_[ch][:], ycv[ch][:])
                mm(pm[:], ones256[:], bf_[ch][:], start=(ch == 0), stop=(ch == 1))
            cp('act', tM[:], pm[:])
            pq2 = nb()
            for ch in range(2):
                act(bf_[2 + ch][:], ycv[ch][:], AF.Square)
                mm(pq2[:], ones256[:], bf_[2 + ch][:], start=(ch == 0), stop=(ch == 1))
            tt('dve', tS[:], tM[:], tM[:], ALU.mult)
            tt('dve', tV[:], pq2[:], tS[:], ALU.subtract)
            act(tV[:], tV[:], AF.Ln, bias=LN_EPS)
            act(tV[:], tV[:], AF.Exp, scale=-0.5)
            slb = [bf_[0], bf_[1]]
            for ch in range(2):
                tt('dve', ycv[ch][:], ycv[ch][:], tM[:], ALU.subtract)
                tt('dve', ycv[ch][:], ycv[ch][:], tV[:], ALU.mult)
                ts('dve', ycv[ch][:], ycv[ch][:], pp[:, P_LNG + ch:P_LNG + ch + 1], pp[:, P_LNB + ch:P_LNB + ch + 1],
                   ALU.mult, ALU.add)
                act(slb[ch][:], ycv[ch][:], AF.Silu)
            for co in range(2):
                pg = nb()
                proj_fm(pg[:], C_GC + co * 128, 128)
                act(tS[:], pg[:], AF.Silu)
                ppw = nb()
                for ci in range(2):
                    mm(ppw[:], wsm[:, W_PW + ci * 256 + co * 128:W_PW + ci * 256 + (co + 1) * 128], slb[ci][:],
                       start=(ci == 0), stop=(ci == 1))
                stt(mixT[:, co, :], ppw[:], pp[:, P_PWB + co:P_PWB + co + 1], tS[:], ALU.add, ALU.mult)

        def attn_branch(tb):
            t0 = tb * TB
            for i in range(TB // 128):
                pv = nb()
                proj_tm(pv[:, 0:384], i, C_V, 384)
                cp('act', vc[:, tb * 4 + i, :], pv[:, 0:384])
            for hp in range(3):
                qT = a_qT
                pq_ = nb()
                proj_fm(pq_[:], C_Q + hp * 128, 128)
                cp('act', qT[:], pq_[:])
                pk = nb()
                proj_fm(pk[:], C_K + hp * 128, 128)
                cp('dve', kTc[:, hp, t0:t0 + TB], pk[:])
                pg = nb()
                proj_fm(pg[:], C_GS + hp * 128, 128)
                sgs = a_sgs
                act(sgs[:], pg[:], AF.Silu)
                ob = P[3]
                accs = [P[0], P[1]]
                zbs = [P[2], P[2]]
                nS = (t0 + TB) // 128
                steps = [(S_, hh) for S_ in reversed(range(nS)) for hh in range(2)]

                def geom(i):
                    S_, hh = steps[i]
                    return S_, hh, hh * 64, max(0, S_ * 128 - t0), (S_ * 128 >= t0)

                def s1_pe(i):
                    S_, hh, pb, c0, diag = geom(i)
                    zb = zbs[hh]
                    mm(zb[:, c0:TB], kTc[pb:pb + 64, hp, S_ * 128:(S_ + 1) * 128], qT[pb:pb + 64, c0:TB],
                       start=True, stop=not diag)
                    if diag:
                        mm(zb[:, c0:c0 + 128], ident[:], negmask[:], start=False, stop=True)

                def s1_act(i):
                    S_, hh, pb, c0, diag = geom(i)
                    zb = zbs[hh]
                    e_ = a_e[i % 3]
                    sp = a_sp[i % 3]
                    act(e_[:, c0:TB], zb[:, c0:TB], AF.Exp, scale=0.125)
                    act(sp[:, c0:TB], e_[:, c0:TB], AF.Ln, bias=1.0)

                def s2_negU(i):
                    S_, hh, pb, c0, diag = geom(i)
                    sp = a_sp[i % 3]
                    mm(accs[hh][:, c0:TB], negU[:], sp[:, c0:TB], start=(S_ == nS - 1), stop=True)

                def s2_ec(i):
                    S_, hh, pb, c0, diag = geom(i)
                    ec = a_ec[i % 2]
                    act(ec[:, c0:TB], accs[hh][:, c0:TB], AF.Exp)

                def s2_negL(i):
                    S_, hh, pb, c0, diag = geom(i)
                    sp = a_sp[i % 3]
                    mm(accs[hh][:, c0:TB], negL[:], sp[:, c0:TB], start=False, stop=True)

                def s2_att(i):
                    S_, hh, pb, c0, diag = geom(i)
                    e_ = a_e[i % 3]
                    ec = a_ec[i % 2]
                    att = a_att[i % 2]
                    tt('dve', att[:, c0:TB], e_[:, c0:TB], ec[:, c0:TB], ALU.mult)

                def s2_av(i):
                    S_, hh, pb, c0, diag = geom(i)
                    h = 2 * hp + hh
                    att = a_att[i % 2]
                    mm(ob[pb:pb + 64, c0:TB], vc[:, S_, h * 64:(h + 1) * 64], att[:, c0:TB],
                       start=(S_ == nS - 1), stop=(S_ == 0))

                LA = 2
                n_ = len(steps)
                for i in range(min(LA, n_)):
                    s1_pe(i)
                    s1_act(i)
                for i in range(n_):
                    s2_negU(i)
                    s2_ec(i)
                    if i + LA < n_:
                        s1_pe(i + LA)
                    if i >= 1:
                        s2_av(i - 1)
                    s2_negL(i)
                    if i + LA < n_:
                        s1_act(i + LA)
                    s2_att(i)
                s2_av(n_ - 1)
                tt('dve', mixT[:, 2 + hp, :], ob[:], sgs[:], ALU.mult)

        def lerp_fm(ps, npart, hidx, mucol, out):
            cp('dve', rawb[0:npart, 0:1], hist[0:npart, hidx:hidx + 1])
            cp('act', rawb[0:npart, 1:TB + 1], ps)
            ts('dve', t32[10][0:npart, :], rawb[0:npart, 0:TB], pp[0:npart, mucol:mucol + 1], None, ALU.mult)
            stt(out, rawb[0:npart, 1:TB + 1], pq[0:npart, mucol - P_MU:mucol - P_MU + 1], t32[10][0:npart, :], ALU.mult, ALU.add)
            cp('dve', hist[0:npart, hidx:hidx + 1], rawb[0:npart, TB:TB + 1])

        def rwkv_v(l, s, tb):
            pwa = nb()
            proj_fm(pwa[0:64, :], C_WD, 64)
            wa = t32[0]
            lerp_fm(pwa[0:64, :], 64, 9, P_MUWA, wa[0:64, :])
            twa = tbf[0]
            act(twa[0:32, :], wa[0:32, :], AF.Tanh)
            cp('act', twa[32:64, :], wa[32:64, :])
            for hp in range(3):
                pvv = nb()
                proj_fm(pvv[:], C_RV + hp * 128, 128)
                lerp_fm(pvv[:], 128, hp * 3 + 2, P_MU + hp * 3 + 2, VP[:, hp, :])
            if l == 0:
                _dma('sp', vfd[s][tb].rearrange("(c p) t -> p c t", p=128), VP[:])
            else:
                VF = t32[1:4]
                for hp in range(3):
                    _dma('sp', VF[hp][:], vfd[s][tb][hp * 128:(hp + 1) * 128, :])
                vpb = tbf[1:4]
                for hp in range(3):
                    cp('dve', vpb[hp][:], VP[:, hp, :])
                pl = nb()
                for hp in range(3):
                    mm(pl[0:32, :], wsm[:, W_V1 + hp * 32:W_V1 + (hp + 1) * 32], vpb[hp][:], start=(hp == 0), stop=(hp == 2))
                lo = tbf[4]
                cp('act', lo[0:32, :], pl[0:32, :])
                for hp in range(3):
                    pm_ = nb()
                    mm(pm_[:], wsm[0:32, W_V2 + hp * 128:W_V2 + (hp + 1) * 128], lo[0:32, :])
                    act(t32[4][:], pm_[:], AF.Sigmoid, bias=pp[:, P_V0 + hp:P_V0 + hp + 1])
                    tt(EW(), VF[hp][:], VF[hp][:], VP[:, hp, :], ALU.subtract)
                    tt(EW(), VF[hp][:], VF[hp][:], t32[4][:], ALU.mult)
                    tt(EW(), VP[:, hp, :], VP[:, hp, :], VF[hp][:], ALU.add)
        def rwkv_pair(part, hp, sid=1):
            par = hp % 2
            AR, TOK, dGt, sgr = ARp[par], TOKp[par], dGtp[par], sgrp[par]
            BT, KT = (tbf[2], tbf[3]) if par == 0 else (BT1, KT1)
            srk = sm[:, par * 8:par * 8 + 8]
            PB = P[4:7]
            twa = tbf[0]
            if part == 'prep':
                rec[0] = sid
                PTf = PT[:, :].bitcast(F32)
                pbank = (lambda: PTf) if hp >= 2 else nb
                Rt, Kt, a_t, kk, K2, bs, cs, g1, g2 = (t32[1], t32[2], t32[3], t32[4], t32[5], t32[6], t32[7], t32[8], t32[9])
                pr = pbank()
                proj_fm(pr[:], C_RR + hp * 128, 128)
                lerp_fm(pr[:], 128, hp * 3 + 0, P_MU + hp * 3 + 0, Rt[:])
                pk = pbank()
                proj_fm(pk[:], C_RK + hp * 128, 128)
                lerp_fm(pk[:], 128, hp * 3 + 1, P_MU + hp * 3 + 1, Kt[:])
                pw_ = pbank()
                mm(pw_[:], wsm[0:32, W_W2A2 + hp * 128:W_W2A2 + (hp + 1) * 128], twa[0:32, :])
                logw = t32[0]
                act(logw[:], pw_[:], AF.Sigmoid, bias=pp[:, P_W0 + hp:P_W0 + hp + 1])
                ts('dve', logw[:], logw[:], -0.6065306597126334, None, ALU.mult)
                pa_ = pbank()
                mm(pa_[:], wsm[32:64, W_W2A2 + hp * 128:W_W2A2 + (hp + 1) * 128], twa[32:64, :])
                act(a_t[:], pa_[:], AF.Sigmoid, bias=pp[:, P_A0 + hp:P_A0 + hp + 1])
                ts('dve', kk[:], Kt[:], pp[:, P_KKS + hp:P_KKS + hp + 1], None, ALU.mult)
                tt('dve', tbf[1][:], kk[:], kk[:], ALU.mult)
                pn = pbank()
                mm(pn[:], bdones[:], tbf[1][:])
                act(g1[:], pn[:], AF.Sqrt)
                ts('dve', g1[:], g1[:], 1e-12, None, ALU.max)
                recip(g1[:], g1[:])
                tt(EW(), kk[:], kk[:], g1[:], ALU.mult)
                ts('dve', g1[:], a_t[:], pp[:, P_KA + hp:P_KA + hp + 1], pq[:, P_KA - P_MU + hp:P_KA - P_MU + hp + 1], ALU.mult, ALU.add)
                tt(EW(), K2[:], Kt[:], g1[:], ALU.mult)
                tt(EW(), bs[:], kk[:], a_t[:], ALU.mult)
                rkp = tbf[1]
                tt(EW(), rkp[:], Rt[:], K2[:], ALU.mult)
                _op('dve', lambda e, o=cs[:], d0=rmask[:], d1=logw[:]: e.tensor_tensor_scan(o, d0, d1, 0.0, ALU.mult, ALU.add),
                     [rmask[:], logw[:]], [cs[:]])
                v3 = lambda tl: tl[:].rearrange("p (c t) -> p c t", t=128)
                tt(EW(), g1[:], cs[:], logw[:], ALU.subtract)
                act(g1[:], g1[:], AF.Exp)
                stt(AR[:, :, 0, :], v3(kk), -1.0, v3(g1), ALU.mult, ALU.mult)
                act(g2[:], cs[:], AF.Exp)
                tt(EW(), AR[:, :, 1, :], v3(Rt), v3(g2), ALU.mult)
                for ck in range(4):
                    ts('dve', dGt[:, ck, :], E64[:], g2[:, ck * 128 + 127:ck * 128 + 128], None, ALU.mult)
                act(g1[:], cs[:], AF.Exp, scale=-1.0)
                BH, KH, VTb = tbf[4], tbf[5], tbf[6]
                tt(EW(), BT[:], bs[:], g1[:], ALU.mult)
                tt(EW(), KT[:], K2[:], g1[:], ALU.mult)
                cend = bass.AP(cs, 127, [[TB, 128], [128, 4], [0, 128]])
                tt(EW(), v3(g1), cend, v3(cs), ALU.subtract)
                act(g1[:], g1[:], AF.Exp)
                tt(EW(), BH[:], bs[:], g1[:], ALU.mult)
                tt(EW(), KH[:], K2[:], g1[:], ALU.mult)
                cp('dve', VTb[:], VP[:, hp, :])
                psr = PTf if hp >= 2 else PB[0]
                for ck in range(4):
                    cs_ = slice(ck * 128, (ck + 1) * 128)
                    tr(PT[:, 0:128], BH[:, cs_], ident[:])
                    tr(PT[:, 128:256], KH[:, cs_], ident[:])
                    tr(PT[:, 256:384], VTb[:, cs_], ident[:])
                    cp('act', TOK[:, ck, :, :], PT[:, 0:384].rearrange("p (a b) -> p a b", b=128))
                    pg = PTf if hp >= 2 else PB[1 + ck % 2]
                    proj_tm(pg[:, 0:128], ck, C_GR + hp * 128, 128)
                    act(sgr[:, ck, :], pg[:, 0:128], AF.Silu)
                    mm(psr[:, ck * 2:(ck + 1) * 2], rkp[:, cs_], wsm[:, W_RKM + hp * 2:W_RKM + hp * 2 + 2])
                    cp('act', sm[:, par * 8 + ck * 2:par * 8 + ck * 2 + 2], psr[:, ck * 2:(ck + 1) * 2])
                return
            rec[0] = 2
            if True:
                for hh in range(2):
                    pb = hh * 64
                    for ck in range(4):
                        ix = hh * 4 + ck
                        cs_ = slice(ck * 128, (ck + 1) * 128)
                        bx = nb()
                        by = nb()
                        mm(bx[:, 0:128], AR[pb:pb + 64, ck, 0, :], BT[pb:pb + 64, cs_])
                        mm(bx[:, 128:384], BT[pb:pb + 64, cs_], AR[pb:pb + 64, ck, :, :])
                        mm(by[:, 0:256], KT[pb:pb + 64, cs_], AR[pb:pb + 64, ck, :, :])
                        tt('dve', QQ[:, ix, :, :], bx[:, 0:256].rearrange("p (a b) -> p a b", b=128),
                           mX[:, 0:256].rearrange("p (a b) -> p a b", b=128), ALU.mult)
                        tt('dve', CM[:, ix, 0, :], bx[:, 256:384], mX[:, 256:384], ALU.mult)
                        tt('dve', CM[:, ix, 1:3, :], by[:, 0:256].rearrange("p (a b) -> p a b", b=128),
                           mX[:, 128:384].rearrange("p (a b) -> p a b", b=128), ALU.mult)
                        tt(EW(), Zt[:, ix, :], QQ[:, ix, 1, :], ident[:], ALU.add)
                for lvl in range(1, 7):
                    last = (lvl == 6)
                    for hh in range(2):
                        bsq = [PB[0], PB[1]]
                        bz = PB[2]
                        for ck in range(4):
                            ix = hh * 4 + ck
                            b_ = bsq[ck // 2]
                            o_ = (ck % 2) * 256
                            mm(b_[:, o_:o_ + 128], QQ[:, ix, 1, :], QQ[:, ix, 0, :])
                            if not last:
                                mm(b_[:, o_ + 128:o_ + 256], QQ[:, ix, 0, :], QQ[:, ix, 1, :])
                        for j in range(2):
                            ix0 = hh * 4 + j * 2
                            cp('act', QQ[:, ix0:ix0 + 2, :, :], bsq[j][:].rearrange("p (a b c) -> p a b c", b=2, c=128))
                        for ck in range(4):
                            ix = hh * 4 + ck
                            mm(bz[:, ck * 128:(ck + 1) * 128], QQ[:, ix, 0, :], Zt[:, ix, :])
                        tt('dve', Zt[:, hh * 4:hh * 4 + 4, :], Zt[:, hh * 4:hh * 4 + 4, :],
                           bz[:].rearrange("p (a b) -> p a b", b=128), ALU.add)
                for ck in range(4):
                    bX = PB[0]
                    for hh in range(2):
                        pb = hh * 64
                        ix = hh * 4 + ck
                        mm(bX[:, hh * 64:(hh + 1) * 64], AR[pb:pb + 64, ck, 0, :], Hs[pb:pb + 64, hp, :], start=True, stop=False)
                        mm(bX[:, hh * 64:(hh + 1) * 64], CM[:, ix, 1, :], TOK[:, ck, 2, hh * 64:(hh + 1) * 64], start=False, stop=True)
                    cp('act', XU[:, 0, :], bX[:, 0:128])
                    bU = PB[1]
                    for hh in range(2):
                        ix = hh * 4 + ck
                        mm(bU[:, hh * 64:(hh + 1) * 64], Zt[:, ix, :], XU[:, 0, hh * 64:(hh + 1) * 64])
                    cp('dve', XU[:, 1, :], bU[:, 0:128])
                    bY = PB[2]
                    bH = PB[0]
                    for hh in range(2):
                        pb = hh * 64
                        ix = hh * 4 + ck
                        hs_ = slice(hh * 64, (hh + 1) * 64)
                        mm(bY[:, hs_], AR[pb:pb + 64, ck, 1, :], Hs[pb:pb + 64, hp, :], start=True, stop=False)
                        mm(bY[:, hs_], CM[:, ix, 0, :], XU[:, 1, hs_], start=False, stop=False)
                        mm(bY[:, hs_], CM[:, ix, 2, :], TOK[:, ck, 2, hs_], start=False, stop=True)
                    for hh in range(2):
                        pb = hh * 64
                        hs_ = slice(hh * 64, (hh + 1) * 64)
                        mm(bH[pb:pb + 64, 0:64], dGt[pb:pb + 64, ck, :], Hs[pb:pb + 64, hp, :], start=True, stop=False)
                        mm(bH[pb:pb + 64, 0:64], TOK[:, ck, 0, hs_], XU[:, 1, hs_], start=False, stop=False)
                        mm(bH[pb:pb + 64, 0:64], TOK[:, ck, 1, hs_], TOK[:, ck, 2, hs_], start=False, stop=True)
                    cp('act', Hs[:, hp, :], bH[:, 0:64])
                    cp('act', y_ys[:, ck * 128:(ck + 1) * 128], bY[:, 0:128])
                ys, sq_, dd = y_ys, y_sq, y_dd
                g8 = lambda a: a.rearrange("p (g v) -> p g v", v=64)
                s1, s2, mn, msq, var, rs = (sm[:, 16:24], sm[:, 24:32], sm[:, 32:40], sm[:, 40:48], sm[:, 48:56], sm[:, 56:64])
                act(sq_[:], ys[:], AF.Square)
                _op('dve', lambda e, o=s1, i=g8(ys[:]): e.reduce_sum(o, i, AX.X), [ys[:]], [s1])
                _op('dve', lambda e, o=s2, i=g8(sq_[:]): e.reduce_sum(o, i, AX.X), [sq_[:]], [s2])
                ts('dve', mn, s1, 1.0 / 64, None, ALU.mult)
                tt('dve', msq, mn, mn, ALU.mult)
                stt(var, s2, 1.0 / 64, msq, ALU.mult, ALU.subtract)
                act(var, var, AF.Ln, bias=GN_EPS)
                act(rs, var, AF.Exp, scale=-0.5)
                bc = lambda a: a.unsqueeze(2).broadcast_to([128, 8, 64])
                tt('dve', g8(dd[:]), g8(ys[:]), bc(mn), ALU.subtract)
                tt('dve', g8(dd[:]), g8(dd[:]), bc(rs), ALU.mult)
                c4 = lambda a: a.rearrange("p (c f) -> p c f", f=128)
                gbc = lambda o: bass.AP(fb, o + hp * 128, [[768, 128], [0, 4], [1, 128]])
                tt('dve', c4(dd[:]), c4(dd[:]), gbc(0), ALU.mult)
                tt('dve', c4(dd[:]), c4(dd[:]), gbc(384), ALU.add)
                tt('dve', sq_[:].rearrange("p (c h v) -> p c h v", h=2, v=64),
                   TOK[:, :, 2, :].rearrange("p c (h v) -> p c h v", v=64),
                   srk.rearrange("p (c h) -> p c h", h=2).unsqueeze(3).broadcast_to([128, 4, 2, 64]), ALU.mult)
                tt('dve', dd[:], dd[:], sq_[:], ALU.add)
                fin4 = y_sq
                tt('dve', c4(fin4[:]), c4(dd[:]), sgr[:, :, :], ALU.mult)
                pfin = PB[1]
                for ck in range(4):
                    tr(pfin[:, ck * 128:(ck + 1) * 128], fin4[:, ck * 128:(ck + 1) * 128], ident32[:])
                cp('act', mixT[:, 5 + hp, :], pfin[:, 0:512])
            rec[0] = 1

        def src_of(l, s, tb):
            t0 = tb * TB
            if l == 0:
                return xT[s].rearrange("(c p) t -> p c t", p=128)[:, :, t0:t0 + TB]
            return x1[s][tb].rearrange("(c p) t -> p c t", p=128)

        def rmsnorm_into_hT(src, xt_, sq_, tmp_, pss):
            for c in range(KC):
                _dma('sp', xt_[c][:], src[:, c, :])
            for c in range(KC):
                sq = sq_[c % len(sq_)]
                act(sq[:], xt_[c][:], AF.Square)
                mm(pss[:], onesD[:], sq[:], start=(c == 0), stop=(c == KC - 1))
            act(tmp_, pss[:], AF.Ln, bias=RMS_EPS)
            act(tmp_, tmp_, AF.Exp, scale=-0.5)
            for c in range(KC):
                stt(hT[:, c, :], xt_[c][:], pp[:, P_PRE + c:P_PRE + c + 1], tmp_, ALU.mult, ALU.mult)

        def run_block(l, s, tb, have_h, nxt):
            t0 = tb * TB
            src = src_of(l, s, tb)
            if l == NL - 1:
                dst = yT[s].rearrange("(c p) t -> p c t", p=128)[:, :, t0:t0 + TB]
            else:
                dst = x1[s][tb].rearrange("(c p) t -> p c t", p=128)
            if not have_h:
                rmsnorm_into_hT(src, t32[0:8], tbf[0:4], t32[9][:], nb())
            rec[0] = 0
            bpool[0] = P[0:4]
            attn_branch(tb)
            rec[0] = 1
            bpool[0] = P[4:7]
            rwkv_v(l, s, tb)
            rwkv_pair('prep', 0)
            rwkv_pair('prep', 1, 3)
            rwkv_pair('b2', 0)
            rwkv_pair('prep', 2, 3)
            rec[0] = 3
            conv_branch()
            rwkv_pair('b2', 1)
            rwkv_pair('b2', 2)
            rec[0] = None
            bpool[0] = P[0:7]
            flush_sched()
            if dbg is not None and dbg == l and s == 0:
                _dma('sp', dbgd.rearrange("(c p) t -> p c t", p=128)[:, :, t0:t0 + TB], mixT[:])
            rec[0] = 0
            pss = P[6]
            o32 = t32[0:8]
            for dm in range(KC):
                po = P[dm % 6]
                _dma('sp', wob[dm % 2][:], wob_d[l][dm])
                for f in range(KC):
                    mm(po[:], wob[dm % 2][:, f, :], mixT[:, f, :], start=(f == 0), stop=(f == KC - 1))
                cp('act', o32[dm][:], po[:])
                sq = tbf[dm % 4]
                act(sq[:], po[:], AF.Square)
                mm(pss[:], onesD[:], sq[:], start=(dm == 0), stop=(dm == KC - 1))
            act(t32[8][:], pss[:], AF.Ln, bias=RMS_EPS)
            act(t32[9][:], t32[8][:], AF.Exp, scale=-0.5)
            for dm in range(KC):
                xt = t32[10] if dm % 2 == 0 else t32[8]
                _dma('sp', xt[:], src[:, dm, :])
                stt(o32[dm][:], o32[dm][:], pp[:, P_POST + dm:P_POST + dm + 1], t32[9][:], ALU.mult, ALU.mult)
                tt('dve', o32[dm][:], o32[dm][:], xt[:], ALU.add)
                _dma('sp', dst[:, dm, :], o32[dm][:])
            if nxt is not None:
                rec[0] = 1
                rmsnorm_into_hT(src_of(l, nxt[0], nxt[1]),
                                [a_e[0], a_e[1], a_e[2], a_ec[0], a_ec[1], y_ys, y_sq, y_dd],
                                [a_sp[0], a_sp[1], a_sp[2], a_att[0], a_att[1]],
                                rawb[:, 0:TB], PT[:, :].bitcast(F32))
            rec[0] = None
            flush_sched()

        for l in range(NL):
            load_weights(l)
            order = [(s_, tb_) for s_ in range(NSEQ) for tb_ in range(NB)]
            for bi, (s, tb) in enumerate(order):
                if tb == 0:
                    memset('pool', ubuf[:, :, 0:30], 0.0)
                    memset('pool', hist[:], 0.0)
                    memset('pool', Hs[:], 0.0)
                nxt = order[bi + 1] if bi + 1 < len(order) else None
                run_block(l, s, tb, have_h=(bi > 0), nxt=nxt)
        S.wait_all_dma('sp')
        S.emit()
    return nc


def pack_params(inp, NL=2):
    f = lambda k: np.asarray(inp[k], dtype=np.float32)
    pp = np.zeros((NL, 128, NPP), np.float32)
    wsm = np.zeros((NL, 128, NWS), np.float32)
    fb = np.zeros((NL, 768), np.float32)
    for l in range(NL):
        pp[l, :, P_PRE:P_PRE + 8] = f('pre_norm_g')[l].reshape(8, 128).T
        pp[l, :, P_POST:P_POST + 8] = f('post_norm_g')[l].reshape(8, 128).T
        dw = f('conv_dw')[l]
        for ch in range(2):
            pp[l, :, P_DW + ch * TAPS:P_DW + (ch + 1) * TAPS] = dw[:, ch * 128:(ch + 1) * 128].T
        for key, col in (('conv_dw_b', P_DWB), ('conv_ln_g', P_LNG), ('conv_ln_b', P_LNB), ('conv_pw_b', P_PWB)):
            pp[l, :, col:col + 2] = f(key)[l].reshape(2, 128).T
        mu = f('rwkv_mu')[l]
        for hp in range(3):
            for fi in range(3):
                pp[l, :, P_MU + hp * 3 + fi] = mu[fi * 384 + hp * 128:fi * 384 + (hp + 1) * 128]
        pp[l, 0:64, P_MUWA] = mu[1152:1216]
        for key, col in (('rwkv_w0', P_W0), ('rwkv_a0', P_A0), ('rwkv_kk_scale', P_KKS), ('rwkv_ka', P_KA)):
            pp[l, :, col:col + 3] = f(key)[l].reshape(3, 128).T
        if l >= 1:
            pp[l, :, P_V0:P_V0 + 3] = f('rwkv_v0')[l - 1].reshape(3, 128).T
        pw = f('conv_pw')[l]
        for ci in range(2):
            wsm[l, :, W_PW + ci * 256:W_PW + (ci + 1) * 256] = pw[ci * 128:(ci + 1) * 128, :]
        wsm[l, 0:32, W_W2A2:W_W2A2 + 384] = f('rwkv_w2')[l]
        wsm[l, 32:64, W_W2A2:W_W2A2 + 384] = f('rwkv_a2')[l]
        if l >= 1:
            v1 = f('rwkv_v1')[l - 1]
            for hp in range(3):
                wsm[l, :, W_V1 + hp * 32:W_V1 + (hp + 1) * 32] = v1[hp * 128:(hp + 1) * 128, :]
            wsm[l, 0:32, W_V2:W_V2 + 384] = f('rwkv_v2')[l - 1]
        rk = f('rwkv_rk')[l].reshape(384)
        for hp in range(3):
            wsm[l, 0:64, W_RKM + hp * 2 + 0] = rk[hp * 128:hp * 128 + 64]
            wsm[l, 64:128, W_RKM + hp * 2 + 1] = rk[hp * 128 + 64:hp * 128 + 128]
        fb[l, 0:384] = f('rwkv_gn_g')[l]
        fb[l, 384:768] = f('rwkv_gn_b')[l]
    return pp, wsm, fb


_NC_CACHE = {}


def kernel(**inputs):
    x = np.asarray(inputs['x'], dtype=np.float32)
    B, T, _ = x.shape
    ncores = 8
    nseq = B // ncores
    pp, wsm, fb = pack_params(inputs)
    w_in = np.ascontiguousarray(np.asarray(inputs['w_in'], dtype=np.float32))
    w_out = np.ascontiguousarray(np.asarray(inputs['w_out'], dtype=np.float32))
    xT = np.ascontiguousarray(x.transpose(0, 2, 1))
    key = (T, nseq)
    if key not in _NC_CACHE:
        _NC_CACHE[key] = build(T=T, NSEQ=nseq, NL=2)
    nc = _NC_CACHE[key]
    in_maps = [{"xT": xT[c * nseq:(c + 1) * nseq], "w_in": w_in, "w_out": w_out, "pp": pp, "wsm": wsm, "fb": fb}
               for c in range(ncores)]
    res = run_bass_kernel_spmd(nc, in_maps, core_ids=list(range(ncores)))
    yT = np.concatenate([r["yT"] for r in res.results], axis=0)
    return np.ascontiguousarray(yT.transpose(0, 2, 1)).astype(np.float32)
```

```python
import contextlib
import numpy as np
import concourse.bass as bass
import concourse.mybir as mybir
from concourse.bass_utils import run_bass_kernel_spmd

F32 = mybir.dt.float32
BF16 = mybir.dt.bfloat16
AF = mybir.ActivationFunctionType
ALU = mybir.AluOpType
AX = mybir.AxisListType

ENGS = ['pe', 'act', 'dve', 'pool', 'sp']
NDMASEM = 24
EPOCH = 30000

D = 1024
KC = 8
TB = 512
HD = 64
CONVW = 256
TAPS = 31
D_IN = 3904
C_GA, C_GB, C_GC, C_Q, C_K, C_V, C_GS = 0, 256, 512, 768, 1152, 1536, 1920
C_RR, C_RK, C_RV, C_WD, C_AD, C_GR = 2304, 2688, 3072, 3456, 3488, 3520
RMS_EPS = 1e-6
LN_EPS = 1e-5
GN_EPS = 64e-5
P_PRE, P_POST, P_DW, P_DWB, P_LNG, P_LNB, P_PWB = 0, 8, 16, 78, 80, 82, 84
P_MU, P_W0, P_A0, P_KKS, P_KA, P_V0, P_MUWA, NPP = 86, 95, 98, 101, 104, 107, 110, 112
W_PW, W_W2A2, W_V1, W_V2, W_RKM, NWS = 0, 512, 896, 992, 1376, 1384


def _box(ap):
    t = ap.tensor
    name = t.name
    pat = [list(x) for x in ap.ap]
    off = int(ap.offset)
    tn = type(t).__name__
    if 'PSum' in tn:
        return (name, 0, 128, 0, 1 << 30)
    if 'DRam' in tn:
        ext = 1
        for s, c in pat:
            ext += (c - 1) * abs(s)
        return (name, 0, 1, off, off + ext)
    F = 1
    for s in list(t.shape)[1:]:
        F *= s
    p0 = off // F
    f0 = off % F
    pstep, pcnt = pat[0]
    pc = 1 if pstep == 0 else (pcnt - 1) * (pstep // F) + 1
    ext = 1
    for s, c in pat[1:]:
        ext += (c - 1) * abs(s)
    return (name, p0, p0 + pc, f0, f0 + ext)


def _ovl(a, b):
    return a[1] < b[2] and b[1] < a[2] and a[3] < b[4] and b[3] < a[4]


def _contains(a, b):
    return a[1] <= b[1] and b[2] <= a[2] and a[3] <= b[3] and b[4] <= a[4]


class Sched:
    def __init__(self, nc):
        self.nc = nc
        self.items = {e: [] for e in ENGS}
        self.count = {e: 0 for e in ENGS}
        self.known = {e: {f: 0 for f in ENGS} for e in ENGS}
        self.opknown = {e: [None] for e in ENGS}
        self.dma_known = {e: set() for e in ENGS}
        self.acc = {}
        self.ndma = 0
        self.dma_info = []

    def _deps(self, reads, writes):
        deps = []
        rb = [_box(a) for a in reads]
        wb = [_box(a) for a in writes]
        for b in rb:
            for rec in self.acc.get(b[0], ()):
                if rec[3] and _ovl(rec[0], b):
                    deps.append(rec)
        for b in wb:
            for rec in self.acc.get(b[0], ()):
                if _ovl(rec[0], b):
                    deps.append(rec)
        return deps, rb, wb

    def _record(self, eng, idx, dma_id, rb, wb):
        for b in wb:
            lst = self.acc.setdefault(b[0], [])
            lst[:] = [r for r in lst if not _contains(b, r[0])]
            lst.append((b, eng, idx, True, dma_id))
        for b in rb:
            lst = self.acc.setdefault(b[0], [])
            if dma_id is None:
                lst[:] = [r for r in lst if not (r[0] == b and r[1] == eng and not r[3] and r[4] is None)]
            lst.append((b, eng, idx, False, dma_id))

    def _emit_waits(self, eng, deps):
        kn = self.known[eng]
        for (b, e2, idx, isw, dma_id) in deps:
            if dma_id is not None:
                if dma_id in self.dma_known[eng]:
                    continue
                slot, val = self.dma_info[dma_id]
                self.items[eng].append(('wait', ('dma', slot), val))
                self.dma_known[eng].add(dma_id)
                continue
            if e2 == eng and eng == 'pe':
                continue
            if kn[e2] >= idx:
                continue
            self.items[eng].append(('wait', ('eng', e2, (idx - 1) // EPOCH), ((idx - 1) % EPOCH) + 1))
            kn[e2] = idx
            ok = self.opknown[e2][idx]
            for f in ENGS:
                if f != eng and ok[f] > kn[f]:
                    kn[f] = ok[f]

    def op(self, eng, fn, reads=(), writes=()):
        deps, rb, wb = self._deps(reads, writes)
        self._emit_waits(eng, deps)
        self.count[eng] += 1
        idx = self.count[eng]
        self.opknown[eng].append(dict(self.known[eng]))
        self.items[eng].append(('op', fn, ('eng', eng, (idx - 1) // EPOCH)))
        self._record(eng, idx, None, rb, wb)

    def dma(self, eng, out, in_, **kw):
        deps, rb, wb = self._deps([in_], [out])
        self._emit_waits(eng, deps)
        i = self.ndma
        self.ndma += 1
        slot = i % NDMASEM
        val = 16 * (i // NDMASEM + 1)
        if i >= NDMASEM and (i - NDMASEM) not in self.dma_known[eng]:
            self.items[eng].append(('wait', ('dma', slot), val - 16))
            self.dma_known[eng].add(i - NDMASEM)
        self.dma_info.append((slot, val))
        self.items[eng].append(('dma', (lambda e, o=out, n=in_, k=kw: e.dma_start(out=o, in_=n, **k)), ('dma', slot)))
        self._record(eng, None, i, rb, wb)
        return i

    def wait_all_dma(self, eng):
        last = {}
        for i, (slot, val) in enumerate(self.dma_info):
            last[slot] = val
        for slot, val in last.items():
            self.items[eng].append(('wait', ('dma', slot), val))

    def emit(self):
        nc = self.nc
        with contextlib.ExitStack() as st:
            sems = {}
            for e in ENGS:
                nep = (self.count[e] + EPOCH - 1) // EPOCH
                for k in range(max(nep, 1)):
                    sems[('eng', e, k)] = st.enter_context(nc.semaphore(f"s_{e}_{k}"))
            for s in range(NDMASEM):
                sems[('dma', s)] = st.enter_context(nc.semaphore(f"s_dma_{s}"))
            block = st.enter_context(nc.Block())
            items = self.items

            def run(engname, engobj):
                for it in items[engname]:
                    if it[0] == 'wait':
                        engobj.wait_ge(sems[it[1]], it[2])
                    elif it[0] == 'op':
                        it[1](engobj).then_inc(sems[it[2]], 1)
                    else:
                        it[1](engobj).then_inc(sems[it[2]], 16)

            @block.tensor
            def _(e):
                run('pe', e)

            @block.scalar
            def _(e):
                run('act', e)

            @block.vector
            def _(e):
                run('dve', e)

            @block.gpsimd
            def _(e):
                run('pool', e)

            @block.sync
            def _(e):
                run('sp', e)


def build(T=2048, NSEQ=2, NL=2, dbg=None, phases=('conv', 'attn', 'rwkv')):
    NB = T // TB
    nc = bass.Bass("TRN2", target_bir_lowering=False)
    dt = nc.dram_tensor
    xT = dt("xT", [NSEQ, D, T], F32, kind="ExternalInput").ap()
    yT = dt("yT", [NSEQ, D, T], F32, kind="ExternalOutput").ap()
    w_in = dt("w_in", [NL, D, D_IN], F32, kind="ExternalInput").ap()
    w_out = dt("w_out", [NL, D, D], F32, kind="ExternalInput").ap()
    ppd = dt("pp", [NL, 128, NPP], F32, kind="ExternalInput").ap()
    wsmd = dt("wsm", [NL, 128, NWS], F32, kind="ExternalInput").ap()
    fbd = dt("fb", [NL, 768], F32, kind="ExternalInput").ap()
    x1 = [[dt(f"x1_{s}_{b}", [D, TB], F32).ap() for b in range(NB)] for s in range(NSEQ)]
    vfd = [[dt(f"vf_{s}_{b}", [384, TB], F32).ap() for b in range(NB)] for s in range(NSEQ)]
    wob_d = [dt(f"wobd{l}", [KC, 128, KC, 128], BF16).ap() for l in range(NL)]
    if dbg is not None:
        dbgd = dt("dbg", [D, T], BF16, kind="ExternalOutput").ap()

    with contextlib.ExitStack() as st:
        def sb(n, s, d):
            return st.enter_context(nc.sbuf_tensor(n, s, d))
        S = Sched(nc)

        win = sb("win", [128, KC, D_IN], BF16)
        wob = [sb(f"wob{i}", [128, KC, 128], BF16) for i in range(2)]
        wsm = sb("wsm_bf", [128, NWS], BF16)
        pp = sb("pp_sb", [128, NPP], F32)
        pq = sb("pq_sb", [128, NPP - P_MU], F32)
        fb = sb("fb_sb", [128, 768], F32)
        ident = sb("ident", [128, 128], BF16)
        ident32 = sb("ident32", [128, 128], F32)
        negmask = sb("negmask", [128, 128], BF16)
        negU = sb("negU", [128, 128], BF16)
        negL = sb("negL", [128, 128], BF16)
        mX = sb("mX", [128, 384], F32)
        onesD = sb("onesD", [128, 128], BF16)
        ones256 = sb("ones256", [128, 128], BF16)
        bdones = sb("bdones", [128, 128], BF16)
        rmask = sb("rmask", [128, TB], F32)
        E64 = sb("E64", [128, 64], F32)
        kTc = sb("kTc", [128, 3, T], BF16)
        vc = sb("vc", [128, T // 128, 384], BF16)
        hT = sb("hT", [128, KC, TB], BF16)
        mixT = sb("mixT", [128, KC, TB], BF16)
        ubuf = sb("ubuf", [128, 2, TB + 30], BF16)
        hist = sb("hist", [128, 16], F32)
        Hs = sb("Hs", [128, 3, 64], BF16)
        NT32 = 11
        t32 = [sb(f"t32_{i}", [128, TB], F32) for i in range(NT32)]
        NTBF = 7
        tbf = [sb(f"tbf_{i}", [128, TB], BF16) for i in range(NTBF)]
        rawb = sb("rawb", [128, TB + 1], F32)
        VP = sb("VP", [128, 3, TB], F32)
        ARp = [sb(f"AR{i}", [128, 4, 2, 128], BF16) for i in range(2)]
        TOKp = [sb(f"TOK{i}", [128, 4, 3, 128], BF16) for i in range(2)]
        BT1 = sb("BT1", [128, TB], BF16)
        KT1 = sb("KT1", [128, TB], BF16)
        y_ys = sb("y_ys", [128, TB], F32)
        y_sq = sb("y_sq", [128, TB], F32)
        y_dd = sb("y_dd", [128, TB], F32)
        CM = sb("CM", [128, 8, 3, 128], BF16)
        QQ = sb("QQ", [128, 8, 2, 128], BF16)
        Zt = sb("Zt", [128, 8, 128], BF16)
        dGtp = [sb(f"dGt{i}", [128, 4, 64], BF16) for i in range(2)]
        sgrp = [sb(f"sgr{i}", [128, 4, 128], BF16) for i in range(2)]
        sm = sb("sm", [128, 64], F32)
        XU = sb("XU", [128, 2, 128], BF16)
        a_sgs = sb("a_sgs", [128, TB], BF16)
        a_qT = sb("a_qT", [128, TB], BF16)
        a_e = [sb(f"a_e{i}", [128, TB], F32) for i in range(3)]
        a_sp = [sb(f"a_sp{i}", [128, TB], BF16) for i in range(3)]
        a_ec = [sb(f"a_ec{i}", [128, TB], F32) for i in range(2)]
        a_att = [sb(f"a_att{i}", [128, TB], BF16) for i in range(2)]
        P = [st.enter_context(nc.psum_tensor(f"P{i}", [128, 512], F32)) for i in range(7)]
        PT = st.enter_context(nc.psum_tensor("PT", [128, 1024], BF16))
        bank_i = [0]
        bpool = [P[0:7]]
        rec = [None]

        def nb():
            bl = bpool[0]
            b = bl[bank_i[0] % len(bl)]
            bank_i[0] += 1
            return b

        recl = []

        def _op(eng, fn, reads=(), writes=()):
            if rec[0] is not None:
                recl.append((rec[0], ('op', eng, fn, list(reads), list(writes))))
            else:
                S.op(eng, fn, reads, writes)

        def _dma(eng, out, in_):
            if rec[0] is not None:
                recl.append((rec[0], ('dma', eng, out, in_)))
            else:
                S.dma(eng, out, in_)

        def _fsize(ap):
            n = 1
            for st_, cnt in list(ap.ap)[1:]:
                n *= cnt
            return n

        def flush_sched():
            n = len(recl)
            accd = {}
            deps_all, durs, engs, sids = [], [], [], []
            for idx, (sid, it) in enumerate(recl):
                sids.append(sid)
                if it[0] == 'op':
                    eng, reads, writes = it[1], it[3], it[4]
                else:
                    eng, reads, writes = 'sp', [it[3]], [it[2]]
                rb = [_box(a_) for a_ in reads]
                wb = [_box(a_) for a_ in writes]
                dp = set()
                for bx_ in rb:
                    for r in accd.get(bx_[0], ()):
                        if r[2] and _ovl(r[0], bx_):
                            dp.add(r[1])
                for bx_ in wb:
                    for r in accd.get(bx_[0], ()):
                        if _ovl(r[0], bx_):
                            dp.add(r[1])
                for bx_ in wb:
                    l_ = accd.setdefault(bx_[0], [])
                    l_[:] = [r for r in l_ if not _contains(bx_, r[0])]
                    l_.append((bx_, idx, True))
                for bx_ in rb:
                    l_ = accd.setdefault(bx_[0], [])
                    l_[:] = [r for r in l_ if not (r[0] == bx_ and not r[2] and sids[r[1]] == sid)]
                    l_.append((bx_, idx, False))
                nel = _fsize(writes[0]) if writes else 64
                if eng == 'pe':
                    d_ = max(nel, 64) / 1400.0
                elif eng == 'act':
                    d_ = nel / 1200.0 + 0.2
                elif eng == 'dve':
                    d_ = nel / 960.0 + 0.12
                elif eng == 'pool':
                    d_ = nel / 450.0 + 0.15
                else:
                    d_ = 2.5
                deps_all.append(dp)
                durs.append(d_)
                engs.append(eng)
            nstream = (max(sids) + 1) if sids else 0
            queues = [[i for i in range(n) if sids[i] == k] for k in range(nstream)]
            ptr = [0] * nstream
            fin = [0.0] * n
            done = [False] * n
            efree = {e: 0.0 for e in ENGS}
            left = n
            filler_run = [0]
            while left:
                best = None
                fallback = None
                for k in range(nstream):
                    if ptr[k] >= len(queues[k]):
                        continue
                    i = queues[k][ptr[k]]
                    ok = True
                    ready = 0.0
                    for j in deps_all[i]:
                        if not done[j]:
                            ok = False
                            break
                        t_ = fin[j] + (0.15 if engs[j] == engs[i] else 0.8)
                        if t_ > ready:
                            ready = t_
                    if not ok:
                        continue
                    start = max(ready, efree[engs[i]])
                    if k == 3 and engs[i] == 'dve' and filler_run[0] >= 2:
                        if fallback is None:
                            fallback = (start, k, i)
                        continue
                    if best is None or start < best[0]:
                        best = (start, k, i)
                if best is None:
                    best = fallback
                start, k, i = best
                if engs[i] == 'dve':
                    filler_run[0] = filler_run[0] + 1 if k == 3 else 0
                fin[i] = start + durs[i]
                efree[engs[i]] = fin[i]
                done[i] = True
                ptr[k] += 1
                left -= 1
                it = recl[i][1]
                if it[0] == 'op':
                    S.op(it[1], it[2], it[3], it[4])
                else:
                    S.dma(it[1], it[2], it[3])
            del recl[:]

        def mm(out, lhsT, rhs, start=True, stop=True):
            _op('pe', lambda e: e.matmul(out, lhsT, rhs, start=start, stop=stop), [lhsT, rhs], [out])

        def tr(out, in_, idn):
            _op('pe', lambda e: e.transpose(out, in_, idn), [in_, idn], [out])

        def act(out, in_, func, bias=None, scale=1.0):
            rd = [in_]
            kw = {}
            if bias is not None:
                kw['bias'] = bias
                if not isinstance(bias, float):
                    rd.append(bias)
            _op('act', lambda e: e.activation(out, in_, func, scale=scale, **kw), rd, [out])

        def tt(eng, out, a, b, op):
            _op(eng, lambda e: e.tensor_tensor(out, a, b, op), [a, b], [out])

        def ts(eng, out, a, s1, s2, op0, op1=None):
            rd = [a] + [s for s in (s1, s2) if s is not None and not isinstance(s, float)]
            if op1 is None and eng == 'pool' and op0 == ALU.mult:
                _op(eng, lambda e: e.tensor_scalar(out, a, s1, 0.0, ALU.mult, ALU.add), rd, [out])
            elif op1 is None:
                _op(eng, lambda e: e.tensor_scalar(out, a, s1, None, op0), rd, [out])
            else:
                _op(eng, lambda e: e.tensor_scalar(out, a, s1, s2, op0, op1), rd, [out])

        def stt(out, a, s, b, op0, op1):
            rd = [a, b] + ([] if isinstance(s, float) else [s])
            _op('dve', lambda e: e.scalar_tensor_tensor(out, a, s, b, op0, op1), rd, [out])

        def cp(eng, out, in_):
            if eng == 'act':
                _op('act', lambda e: e.copy(out, in_), [in_], [out])
            else:
                _op(eng, lambda e: e.tensor_copy(out, in_), [in_], [out])

        ew_i = [0]

        def EW():
            ew_i[0] += 1
            return 'dve'

        def recip(out, in_):
            _op('dve', lambda e: e.reciprocal(out, in_), [in_], [out])

        def memset(eng, out, v):
            _op(eng, lambda e: e.memset(out, v), [], [out])

        def asel(out, in_, pattern, op, fill, base, cm):
            _op('pool', lambda e: e.affine_select(out, in_, pattern, op, fill, base=base, channel_multiplier=cm),
                 [in_], [out])

        memset('pool', ident32[:], 1.0)
        asel(ident32[:], ident32[:], [[-1, 128]], ALU.is_equal, 0.0, 0, 1)
        cp('pool', ident[:], ident32[:])
        memset('pool', t32[0][:, 0:128], -30000.0)
        asel(t32[0][:, 0:128], t32[0][:, 0:128], [[-1, 128]], ALU.is_ge, 0.0, 0, 1)
        cp('pool', negmask[:], t32[0][:, 0:128])
        memset('pool', t32[0][:, 0:128], -1.0)
        asel(t32[0][:, 0:128], t32[0][:, 0:128], [[-1, 128]], ALU.is_ge, 0.0, 0, 1)
        cp('pool', negU[:], t32[0][:, 0:128])
        memset('pool', t32[0][:, 0:128], -1.0)
        asel(t32[0][:, 0:128], t32[0][:, 0:128], [[1, 128]], ALU.is_gt, 0.0, 0, -1)
        cp('pool', negL[:], t32[0][:, 0:128])
        memset('pool', mX[:], 1.0)
        asel(mX[:, 0:128], mX[:, 0:128], [[-1, 128]], ALU.is_gt, 0.0, 0, 1)
        asel(mX[:, 128:256], mX[:, 128:256], [[1, 128]], ALU.is_gt, 0.0, 0, -1)
        asel(mX[:, 256:384], mX[:, 256:384], [[1, 128]], ALU.is_ge, 0.0, 0, -1)
        memset('pool', onesD[:], 1.0 / D)
        memset('pool', ones256[:], 1.0 / CONVW)
        memset('pool', bdones[:], 0.0)
        memset('pool', bdones[0:64, 0:64], 1.0)
        memset('pool', bdones[64:128, 64:128], 1.0)
        memset('pool', rmask[:], 1.0)
        for c in range(TB // 128):
            memset('pool', rmask[:, c * 128:c * 128 + 1], 0.0)
        memset('pool', E64[:], 0.0)
        cp('pool', E64[0:64, :], ident32[0:64, 0:64])
        cp('pool', E64[64:128, :], ident32[64:128, 64:128])

        def load_weights(l):
            _dma('sp', pp[:], ppd[l])
            ts('pool', pq[:], pp[:, P_MU:NPP], -1.0, 1.0, ALU.mult, ALU.add)
            _dma('sp', fb[:], fbd[l:l + 1, :].broadcast_to([128, 768]))
            k = 0
            CW = 488
            stage = list(t32) + list(a_e) + list(a_ec) + [y_ys, y_sq, y_dd]
            ceng = ['pool', 'act', 'dve', 'act', 'dve', 'act']
            NSTG = len(stage)
            for c in range(KC):
                for q in range(D_IN // CW):
                    sg_ = stage[k % NSTG]
                    k += 1
                    _dma('sp', sg_[:, 0:CW], w_in[l, c * 128:(c + 1) * 128, q * CW:(q + 1) * CW])
                    cp(ceng[k % 6], win[:, c, q * CW:(q + 1) * CW], sg_[:, 0:CW])
            for c in range(KC):
                for q in range(2):
                    sg_ = stage[k % NSTG]
                    k += 1
                    _dma('sp', sg_[:, 0:512], w_out[l, c * 128:(c + 1) * 128, q * 512:(q + 1) * 512])
                    wb_ = tbf[k % 6]
                    cp(ceng[k % 6], wb_[:], sg_[:, 0:512])
                    _dma('sp', wob_d[l][q * 4:(q + 1) * 4, :, c, :].rearrange("d p n -> p d n"),
                         wb_[:].rearrange("p (d n) -> p d n", n=128))
            for q in range(4):
                sg_ = stage[k % NSTG]
                k += 1
                _dma('sp', sg_[:, 0:NWS // 4], wsmd[l, :, q * (NWS // 4):(q + 1) * (NWS // 4)])
                cp(ceng[k % 6], wsm[:, q * (NWS // 4):(q + 1) * (NWS // 4)], sg_[:, 0:NWS // 4])

        def proj_fm(ps, col0, M):
            for c in range(KC):
                mm(ps, win[:, c, col0:col0 + M], hT[:, c, :], start=(c == 0), stop=(c == KC - 1))

        def proj_tm(ps, ti, col0, N):
            for c in range(KC):
                mm(ps, hT[:, c, ti * 128:(ti + 1) * 128], win[:, c, col0:col0 + N], start=(c == 0), stop=(c == KC - 1))

        def conv_branch():
            ycv = [t32[1], t32[2]]
            tM, tV, tS = t32[3], t32[4], t32[5]
            bf_ = [tbf[1], tbf[4], tbf[5], tbf[6]]
            PTf = PT[:, :].bitcast(F32)
            nb = lambda: PTf
            for ch in range(2):
                pb_ = nb()
                proj_fm(pb_[:], C_GB + ch * 128, 128)
                act(tS[:], pb_[:], AF.Sigmoid)
                pa = nb()
                proj_fm(pa[:], C_GA + ch * 128, 128)
                tt('dve', ubuf[:, ch, 30:30 + TB], pa[:], tS[:], ALU.mult)
            for ch in range(2):
                ts('dve', ycv[ch][:], ubuf[:, ch, 0:TB], pp[:, P_DW + ch * TAPS:P_DW + ch * TAPS + 1],
                   pp[:, P_DWB + ch:P_DWB + ch + 1], ALU.mult, ALU.add)
            for j in range(1, TAPS):
                for ch in range(2):
                    stt(ycv[ch][:], ubuf[:, ch, j:j + TB], pp[:, P_DW + ch * TAPS + j:P_DW + ch * TAPS + j + 1],
                        ycv[ch][:], ALU.mult, ALU.add)
            for ch in range(2):
                cp('dve', ubuf[:, ch, 0:30], ubuf[:, ch, TB:TB + 30])
            pm = nb()
            for ch in range(2):
                cp('dve', bf_[ch][:], ycv[ch][:])
                mm(pm[:], ones256[:], bf_[ch][:], start=(ch == 0), stop=(ch == 1))
            cp('act', tM[:], pm[:])
            pq2 = nb()
            for ch in range(2):
                act(bf_[2 + ch][:], ycv[ch][:], AF.Square)
                mm(pq2[:], ones256[:], bf_[2 + ch][:], start=(ch == 0), stop=(ch == 1))
            tt('dve', tS[:], tM[:], tM[:], ALU.mult)
            tt('dve', tV[:], pq2[:], tS[:], ALU.subtract)
            act(tV[:], tV[:], AF.Ln, bias=LN_EPS)
            act(tV[:], tV[:], AF.Exp, scale=-0.5)
            slb = [bf_[0], bf_[1]]
            for ch in range(2):
                tt('dve', ycv[ch][:], ycv[ch][:], tM[:], ALU.subtract)
                tt('dve', ycv[ch][:], ycv[ch][:], tV[:], ALU.mult)
                ts('dve', ycv[ch][:], ycv[ch][:], pp[:, P_LNG + ch:P_LNG + ch + 1], pp[:, P_LNB + ch:P_LNB + ch + 1],
                   ALU.mult, ALU.add)
                act(slb[ch][:], ycv[ch][:], AF.Silu)
            for co in range(2):
                pg = nb()
                proj_fm(pg[:], C_GC + co * 128, 128)
                act(tS[:], pg[:], AF.Silu)
                ppw = nb()
                for ci in range(2):
                    mm(ppw[:], wsm[:, W_PW + ci * 256 + co * 128:W_PW + ci * 256 + (co + 1) * 128], slb[ci][:],
                       start=(ci == 0), stop=(ci == 1))
                stt(mixT[:, co, :], ppw[:], pp[:, P_PWB + co:P_PWB + co + 1], tS[:], ALU.add, ALU.mult)

        def attn_branch(tb):
            t0 = tb * TB
            for i in range(TB // 128):
                pv = nb()
                proj_tm(pv[:, 0:384], i, C_V, 384)
                cp('act', vc[:, tb * 4 + i, :], pv[:, 0:384])
            for hp in range(3):
                qT = a_qT
                pq_ = nb()
                proj_fm(pq_[:], C_Q + hp * 128, 128)
                cp('act', qT[:], pq_[:])
                pk = nb()
                proj_fm(pk[:], C_K + hp * 128, 128)
                cp('dve', kTc[:, hp, t0:t0 + TB], pk[:])
                pg = nb()
                proj_fm(pg[:], C_GS + hp * 128, 128)
                sgs = a_sgs
                act(sgs[:], pg[:], AF.Silu)
                ob = P[3]
                accs = [P[0], P[1]]
                zbs = [P[2], P[2]]
                nS = (t0 + TB) // 128
                steps = [(S_, hh) for S_ in reversed(range(nS)) for hh in range(2)]

                def geom(i):
                    S_, hh = steps[i]
                    return S_, hh, hh * 64, max(0, S_ * 128 - t0), (S_ * 128 >= t0)

                def s1_pe(i):
                    S_, hh, pb, c0, diag = geom(i)
                    zb = zbs[hh]
                    mm(zb[:, c0:TB], kTc[pb:pb + 64, hp, S_ * 128:(S_ + 1) * 128], qT[pb:pb + 64, c0:TB],
                       start=True, stop=not diag)
                    if diag:
                        mm(zb[:, c0:c0 + 128], ident[:], negmask[:], start=False, stop=True)

                def s1_act(i):
                    S_, hh, pb, c0, diag = geom(i)
                    zb = zbs[hh]
                    e_ = a_e[i % 3]
                    sp = a_sp[i % 3]
                    act(e_[:, c0:TB], zb[:, c0:TB], AF.Exp, scale=0.125)
                    act(sp[:, c0:TB], e_[:, c0:TB], AF.Ln, bias=1.0)

                def s2_negU(i):
                    S_, hh, pb, c0, diag = geom(i)
                    sp = a_sp[i % 3]
                    mm(accs[hh][:, c0:TB], negU[:], sp[:, c0:TB], start=(S_ == nS - 1), stop=True)

                def s2_ec(i):
                    S_, hh, pb, c0, diag = geom(i)
                    ec = a_ec[i % 2]
                    act(ec[:, c0:TB], accs[hh][:, c0:TB], AF.Exp)

                def s2_negL(i):
                    S_, hh, pb, c0, diag = geom(i)
                    sp = a_sp[i % 3]
                    mm(accs[hh][:, c0:TB], negL[:], sp[:, c0:TB], start=False, stop=True)

                def s2_att(i):
                    S_, hh, pb, c0, diag = geom(i)
                    e_ = a_e[i % 3]
                    ec = a_ec[i % 2]
                    att = a_att[i % 2]
                    tt('dve', att[:, c0:TB], e_[:, c0:TB], ec[:, c0:TB], ALU.mult)

                def s2_av(i):
                    S_, hh, pb, c0, diag = geom(i)
                    h = 2 * hp + hh
                    att = a_att[i % 2]
                    mm(ob[pb:pb + 64, c0:TB], vc[:, S_, h * 64:(h + 1) * 64], att[:, c0:TB],
                       start=(S_ == nS - 1), stop=(S_ == 0))

                LA = 2
                n_ = len(steps)
                for i in range(min(LA, n_)):
                    s1_pe(i)
                    s1_act(i)
                for i in range(n_):
                    s2_negU(i)
                    s2_ec(i)
                    if i + LA < n_:
                        s1_pe(i + LA)
                    if i >= 1:
                        s2_av(i - 1)
                    s2_negL(i)
                    if i + LA < n_:
                        s1_act(i + LA)
                    s2_att(i)
                s2_av(n_ - 1)
                tt('dve', mixT[:, 2 + hp, :], ob[:], sgs[:], ALU.mult)

        def lerp_fm(ps, npart, hidx, mucol, out):
            cp('dve', rawb[0:npart, 0:1], hist[0:npart, hidx:hidx + 1])
            cp('act', rawb[0:npart, 1:TB + 1], ps)
            ts('dve', t32[10][0:npart, :], rawb[0:npart, 0:TB], pp[0:npart, mucol:mucol + 1], None, ALU.mult)
            stt(out, rawb[0:npart, 1:TB + 1], pq[0:npart, mucol - P_MU:mucol - P_MU + 1], t32[10][0:npart, :], ALU.mult, ALU.add)
            cp('dve', hist[0:npart, hidx:hidx + 1], rawb[0:npart, TB:TB + 1])

        def rwkv_v(l, s, tb):
            pwa = nb()
            proj_fm(pwa[0:64, :], C_WD, 64)
            wa = t32[0]
            lerp_fm(pwa[0:64, :], 64, 9, P_MUWA, wa[0:64, :])
            twa = tbf[0]
            act(twa[0:32, :], wa[0:32, :], AF.Tanh)
            cp('act', twa[32:64, :], wa[32:64, :])
            for hp in range(3):
                pvv = nb()
                proj_fm(pvv[:], C_RV + hp * 128, 128)
                lerp_fm(pvv[:], 128, hp * 3 + 2, P_MU + hp * 3 + 2, VP[:, hp, :])
            if l == 0:
                _dma('sp', vfd[s][tb].rearrange("(c p) t -> p c t", p=128), VP[:])
            else:
                VF = t32[1:4]
                for hp in range(3):
                    _dma('sp', VF[hp][:], vfd[s][tb][hp * 128:(hp + 1) * 128, :])
                vpb = tbf[1:4]
                for hp in range(3):
                    cp('dve', vpb[hp][:], VP[:, hp, :])
                pl = nb()
                for hp in range(3):
                    mm(pl[0:32, :], wsm[:, W_V1 + hp * 32:W_V1 + (hp + 1) * 32], vpb[hp][:], start=(hp == 0), stop=(hp == 2))
                lo = tbf[4]
                cp('act', lo[0:32, :], pl[0:32, :])
                for hp in range(3):
                    pm_ = nb()
                    mm(pm_[:], wsm[0:32, W_V2 + hp * 128:W_V2 + (hp + 1) * 128], lo[0:32, :])
                    act(t32[4][:], pm_[:], AF.Sigmoid, bias=pp[:, P_V0 + hp:P_V0 + hp + 1])
                    tt(EW(), VF[hp][:], VF[hp][:], VP[:, hp, :], ALU.subtract)
                    tt(EW(), VF[hp][:], VF[hp][:], t32[4][:], ALU.mult)
                    tt(EW(), VP[:, hp, :], VP[:, hp, :], VF[hp][:], ALU.add)
        def rwkv_pair(part, hp, sid=1):
            par = hp % 2
            AR, TOK, dGt, sgr = ARp[par], TOKp[par], dGtp[par], sgrp[par]
            BT, KT = (tbf[2], tbf[3]) if par == 0 else (BT1, KT1)
            srk = sm[:, par * 8:par * 8 + 8]
            PB = P[4:7]
            twa = tbf[0]
            if part == 'prep':
                rec[0] = sid
                PTf = PT[:, :].bitcast(F32)
                pbank = (lambda: PTf) if hp >= 2 else nb
                Rt, Kt, a_t, kk, K2, bs, cs, g1, g2 = (t32[1], t32[2], t32[3], t32[4], t32[5], t32[6], t32[7], t32[8], t32[9])
                pr = pbank()
                proj_fm(pr[:], C_RR + hp * 128, 128)
                lerp_fm(pr[:], 128, hp * 3 + 0, P_MU + hp * 3 + 0, Rt[:])
                pk = pbank()
                proj_fm(pk[:], C_RK + hp * 128, 128)
                lerp_fm(pk[:], 128, hp * 3 + 1, P_MU + hp * 3 + 1, Kt[:])
                pw_ = pbank()
                mm(pw_[:], wsm[0:32, W_W2A2 + hp * 128:W_W2A2 + (hp + 1) * 128], twa[0:32, :])
                logw = t32[0]
                act(logw[:], pw_[:], AF.Sigmoid, bias=pp[:, P_W0 + hp:P_W0 + hp + 1])
                ts('dve', logw[:], logw[:], -0.6065306597126334, None, ALU.mult)
                pa_ = pbank()
                mm(pa_[:], wsm[32:64, W_W2A2 + hp * 128:W_W2A2 + (hp + 1) * 128], twa[32:64, :])
                act(a_t[:], pa_[:], AF.Sigmoid, bias=pp[:, P_A0 + hp:P_A0 + hp + 1])
                ts('dve', kk[:], Kt[:], pp[:, P_KKS + hp:P_KKS + hp + 1], None, ALU.mult)
                tt('dve', tbf[1][:], kk[:], kk[:], ALU.mult)
                pn = pbank()
                mm(pn[:], bdones[:], tbf[1][:])
                act(g1[:], pn[:], AF.Sqrt)
                ts('dve', g1[:], g1[:], 1e-12, None, ALU.max)
                recip(g1[:], g1[:])
                tt(EW(), kk[:], kk[:], g1[:], ALU.mult)
                ts('dve', g1[:], a_t[:], pp[:, P_KA + hp:P_KA + hp + 1], pq[:, P_KA - P_MU + hp:P_KA - P_MU + hp + 1], ALU.mult, ALU.add)
                tt(EW(), K2[:], Kt[:], g1[:], ALU.mult)
                tt(EW(), bs[:], kk[:], a_t[:], ALU.mult)
                rkp = tbf[1]
                tt(EW(), rkp[:], Rt[:], K2[:], ALU.mult)
                _op('dve', lambda e, o=cs[:], d0=rmask[:], d1=logw[:]: e.tensor_tensor_scan(o, d0, d1, 0.0, ALU.mult, ALU.add),
                     [rmask[:], logw[:]], [cs[:]])
                v3 = lambda tl: tl[:].rearrange("p (c t) -> p c t", t=128)
                tt(EW(), g1[:], cs[:], logw[:], ALU.subtract)
                act(g1[:], g1[:], AF.Exp)
                stt(AR[:, :, 0, :], v3(kk), -1.0, v3(g1), ALU.mult, ALU.mult)
                act(g2[:], cs[:], AF.Exp)
                tt(EW(), AR[:, :, 1, :], v3(Rt), v3(g2), ALU.mult)
                for ck in range(4):
                    ts('dve', dGt[:, ck, :], E64[:], g2[:, ck * 128 + 127:ck * 128 + 128], None, ALU.mult)
                act(g1[:], cs[:], AF.Exp, scale=-1.0)
                BH, KH, VTb = tbf[4], tbf[5], tbf[6]
                tt(EW(), BT[:], bs[:], g1[:], ALU.mult)
                tt(EW(), KT[:], K2[:], g1[:], ALU.mult)
                cend = bass.AP(cs, 127, [[TB, 128], [128, 4], [0, 128]])
                tt(EW(), v3(g1), cend, v3(cs), ALU.subtract)
                act(g1[:], g1[:], AF.Exp)
                tt(EW(), BH[:], bs[:], g1[:], ALU.mult)
                tt(EW(), KH[:], K2[:], g1[:], ALU.mult)
                cp('dve', VTb[:], VP[:, hp, :])
                psr = PTf if hp >= 2 else PB[0]
                for ck in range(4):
                    cs_ = slice(ck * 128, (ck + 1) * 128)
                    tr(PT[:, 0:128], BH[:, cs_], ident[:])
                    tr(PT[:, 128:256], KH[:, cs_], ident[:])
                    tr(PT[:, 256:384], VTb[:, cs_], ident[:])
                    cp('act', TOK[:, ck, :, :], PT[:, 0:384].rearrange("p (a b) -> p a b", b=128))
                    pg = PTf if hp >= 2 else PB[1 + ck % 2]
                    proj_tm(pg[:, 0:128], ck, C_GR + hp * 128, 128)
                    act(sgr[:, ck, :], pg[:, 0:128], AF.Silu)
                    mm(psr[:, ck * 2:(ck + 1) * 2], rkp[:, cs_], wsm[:, W_RKM + hp * 2:W_RKM + hp * 2 + 2])
                    cp('act', sm[:, par * 8 + ck * 2:par * 8 + ck * 2 + 2], psr[:, ck * 2:(ck + 1) * 2])
                return
            rec[0] = 2
            if True:
                for hh in range(2):
                    pb = hh * 64
                    for ck in range(4):
                        ix = hh * 4 + ck
                        cs_ = slice(ck * 128, (ck + 1) * 128)
                        bx = nb()
                        by = nb()
                        mm(bx[:, 0:128], AR[pb:pb + 64, ck, 0, :], BT[pb:pb + 64, cs_])
                        mm(bx[:, 128:384], BT[pb:pb + 64, cs_], AR[pb:pb + 64, ck, :, :])
                        mm(by[:, 0:256], KT[pb:pb + 64, cs_], AR[pb:pb + 64, ck, :, :])
                        tt('dve', QQ[:, ix, :, :], bx[:, 0:256].rearrange("p (a b) -> p a b", b=128),
                           mX[:, 0:256].rearrange("p (a b) -> p a b", b=128), ALU.mult)
                        tt('dve', CM[:, ix, 0, :], bx[:, 256:384], mX[:, 256:384], ALU.mult)
                        tt('dve', CM[:, ix, 1:3, :], by[:, 0:256].rearrange("p (a b) -> p a b", b=128),
                           mX[:, 128:384].rearrange("p (a b) -> p a b", b=128), ALU.mult)
                        tt(EW(), Zt[:, ix, :], QQ[:, ix, 1, :], ident[:], ALU.add)
                for lvl in range(1, 7):
                    last = (lvl == 6)
                    for hh in range(2):
                        bsq = [PB[0], PB[1]]
                        bz = PB[2]
                        for ck in range(4):
                            ix = hh * 4 + ck
                            b_ = bsq[ck // 2]
                            o_ = (ck % 2) * 256
                            mm(b_[:, o_:o_ + 128], QQ[:, ix, 1, :], QQ[:, ix, 0, :])
                            if not last:
                                mm(b_[:, o_ + 128:o_ + 256], QQ[:, ix, 0, :], QQ[:, ix, 1, :])
                        for j in range(2):
                            ix0 = hh * 4 + j * 2
                            cp('act', QQ[:, ix0:ix0 + 2, :, :], bsq[j][:].rearrange("p (a b c) -> p a b c", b=2, c=128))
                        for ck in range(4):
                            ix = hh * 4 + ck
                            mm(bz[:, ck * 128:(ck + 1) * 128], QQ[:, ix, 0, :], Zt[:, ix, :])
                        tt('dve', Zt[:, hh * 4:hh * 4 + 4, :], Zt[:, hh * 4:hh * 4 + 4, :],
                           bz[:].rearrange("p (a b) -> p a b", b=128), ALU.add)
                for ck in range(4):
                    bX = PB[0]
                    for hh in range(2):
                        pb = hh * 64
                        ix = hh * 4 + ck
                        mm(bX[:, hh * 64:(hh + 1) * 64], AR[pb:pb + 64, ck, 0, :], Hs[pb:pb + 64, hp, :], start=True, stop=False)
                        mm(bX[:, hh * 64:(hh + 1) * 64], CM[:, ix, 1, :], TOK[:, ck, 2, hh * 64:(hh + 1) * 64], start=False, stop=True)
                    cp('act', XU[:, 0, :], bX[:, 0:128])
                    bU = PB[1]
                    for hh in range(2):
                        ix = hh * 4 + ck
                        mm(bU[:, hh * 64:(hh + 1) * 64], Zt[:, ix, :], XU[:, 0, hh * 64:(hh + 1) * 64])
                    cp('dve', XU[:, 1, :], bU[:, 0:128])
                    bY = PB[2]
                    bH = PB[0]
                    for hh in range(2):
                        pb = hh * 64
                        ix = hh * 4 + ck
                        hs_ = slice(hh * 64, (hh + 1) * 64)
                        mm(bY[:, hs_], AR[pb:pb + 64, ck, 1, :], Hs[pb:pb + 64, hp, :], start=True, stop=False)
                        mm(bY[:, hs_], CM[:, ix, 0, :], XU[:, 1, hs_], start=False, stop=False)
                        mm(bY[:, hs_], CM[:, ix, 2, :], TOK[:, ck, 2, hs_], start=False, stop=True)
                    for hh in range(2):
                        pb = hh * 64
                        hs_ = slice(hh * 64, (hh + 1) * 64)
                        mm(bH[pb:pb + 64, 0:64], dGt[pb:pb + 64, ck, :], Hs[pb:pb + 64, hp, :], start=True, stop=False)
                        mm(bH[pb:pb + 64, 0:64], TOK[:, ck, 0, hs_], XU[:, 1, hs_], start=False, stop=False)
                        mm(bH[pb:pb + 64, 0:64], TOK[:, ck, 1, hs_], TOK[:, ck, 2, hs_], start=False, stop=True)
                    cp('act', Hs[:, hp, :], bH[:, 0:64])
                    cp('act', y_ys[:, ck * 128:(ck + 1) * 128], bY[:, 0:128])
                ys, sq_, dd = y_ys, y_sq, y_dd
                g8 = lambda a: a.rearrange("p (g v) -> p g v", v=64)
                s1, s2, mn, msq, var, rs = (sm[:, 16:24], sm[:, 24:32], sm[:, 32:40], sm[:, 40:48], sm[:, 48:56], sm[:, 56:64])
                act(sq_[:], ys[:], AF.Square)
                _op('dve', lambda e, o=s1, i=g8(ys[:]): e.reduce_sum(o, i, AX.X), [ys[:]], [s1])
                _op('dve', lambda e, o=s2, i=g8(sq_[:]): e.reduce_sum(o, i, AX.X), [sq_[:]], [s2])
                ts('dve', mn, s1, 1.0 / 64, None, ALU.mult)
                tt('dve', msq, mn, mn, ALU.mult)
                stt(var, s2, 1.0 / 64, msq, ALU.mult, ALU.subtract)
                act(var, var, AF.Ln, bias=GN_EPS)
                act(rs, var, AF.Exp, scale=-0.5)
                bc = lambda a: a.unsqueeze(2).broadcast_to([128, 8, 64])
                tt('dve', g8(dd[:]), g8(ys[:]), bc(mn), ALU.subtract)
                tt('dve', g8(dd[:]), g8(dd[:]), bc(rs), ALU.mult)
                c4 = lambda a: a.rearrange("p (c f) -> p c f", f=128)
                gbc = lambda o: bass.AP(fb, o + hp * 128, [[768, 128], [0, 4], [1, 128]])
                tt('dve', c4(dd[:]), c4(dd[:]), gbc(0), ALU.mult)
                tt('dve', c4(dd[:]), c4(dd[:]), gbc(384), ALU.add)
                tt('dve', sq_[:].rearrange("p (c h v) -> p c h v", h=2, v=64),
                   TOK[:, :, 2, :].rearrange("p c (h v) -> p c h v", v=64),
                   srk.rearrange("p (c h) -> p c h", h=2).unsqueeze(3).broadcast_to([128, 4, 2, 64]), ALU.mult)
                tt('dve', dd[:], dd[:], sq_[:], ALU.add)
                fin4 = y_sq
                tt('dve', c4(fin4[:]), c4(dd[:]), sgr[:, :, :], ALU.mult)
                pfin = PB[1]
                for ck in range(4):
                    tr(pfin[:, ck * 128:(ck + 1) * 128], fin4[:, ck * 128:(ck + 1) * 128], ident32[:])
                cp('act', mixT[:, 5 + hp, :], pfin[:, 0:512])
            rec[0] = 1

        def src_of(l, s, tb):
            t0 = tb * TB
            if l == 0:
                return xT[s].rearrange("(c p) t -> p c t", p=128)[:, :, t0:t0 + TB]
            return x1[s][tb].rearrange("(c p) t -> p c t", p=128)

        def rmsnorm_into_hT(src, xt_, sq_, tmp_, pss):
            for c in range(KC):
                _dma('sp', xt_[c][:], src[:, c, :])
            for c in range(KC):
                sq = sq_[c % len(sq_)]
                act(sq[:], xt_[c][:], AF.Square)
                mm(pss[:], onesD[:], sq[:], start=(c == 0), stop=(c == KC - 1))
            act(tmp_, pss[:], AF.Ln, bias=RMS_EPS)
            act(tmp_, tmp_, AF.Exp, scale=-0.5)
            for c in range(KC):
                stt(hT[:, c, :], xt_[c][:], pp[:, P_PRE + c:P_PRE + c + 1], tmp_, ALU.mult, ALU.mult)

        def run_block(l, s, tb, have_h, nxt):
            t0 = tb * TB
            src = src_of(l, s, tb)
            if l == NL - 1:
                dst = yT[s].rearrange("(c p) t -> p c t", p=128)[:, :, t0:t0 + TB]
            else:
                dst = x1[s][tb].rearrange("(c p) t -> p c t", p=128)
            if not have_h:
                rmsnorm_into_hT(src, t32[0:8], tbf[0:4], t32[9][:], nb())
            rec[0] = 0
            bpool[0] = P[0:4]
            attn_branch(tb)
            rec[0] = 1
            bpool[0] = P[4:7]
            rwkv_v(l, s, tb)
            rwkv_pair('prep', 0)
            rwkv_pair('prep', 1, 3)
            rwkv_pair('b2', 0)
            rwkv_pair('prep', 2, 3)
            rec[0] = 3
            conv_branch()
            rwkv_pair('b2', 1)
            rwkv_pair('b2', 2)
            rec[0] = None
            bpool[0] = P[0:7]
            flush_sched()
            if dbg is not None and dbg == l and s == 0:
                _dma('sp', dbgd.rearrange("(c p) t -> p c t", p=128)[:, :, t0:t0 + TB], mixT[:])
            rec[0] = 0
            pss = P[6]
            o32 = t32[0:8]
            for dm in range(KC):
                po = P[dm % 6]
                _dma('sp', wob[dm % 2][:], wob_d[l][dm])
                for f in range(KC):
                    mm(po[:], wob[dm % 2][:, f, :], mixT[:, f, :], start=(f == 0), stop=(f == KC - 1))
                cp('act', o32[dm][:], po[:])
                sq = tbf[dm % 4]
                act(sq[:], po[:], AF.Square)
                mm(pss[:], onesD[:], sq[:], start=(dm == 0), stop=(dm == KC - 1))
            act(t32[8][:], pss[:], AF.Ln, bias=RMS_EPS)
            act(t32[9][:], t32[8][:], AF.Exp, scale=-0.5)
            for dm in range(KC):
                xt = t32[10] if dm % 2 == 0 else t32[8]
                _dma('sp', xt[:], src[:, dm, :])
                stt(o32[dm][:], o32[dm][:], pp[:, P_POST + dm:P_POST + dm + 1], t32[9][:], ALU.mult, ALU.mult)
                tt('dve', o32[dm][:], o32[dm][:], xt[:], ALU.add)
                _dma('sp', dst[:, dm, :], o32[dm][:])
            if nxt is not None:
                rec[0] = 1
                rmsnorm_into_hT(src_of(l, nxt[0], nxt[1]),
                                [a_e[0], a_e[1], a_e[2], a_ec[0], a_ec[1], y_ys, y_sq, y_dd],
                                [a_sp[0], a_sp[1], a_sp[2], a_att[0], a_att[1]],
                                rawb[:, 0:TB], PT[:, :].bitcast(F32))
            rec[0] = None
            flush_sched()

        for l in range(NL):
            load_weights(l)
            order = [(s_, tb_) for s_ in range(NSEQ) for tb_ in range(NB)]
            for bi, (s, tb) in enumerate(order):
                if tb == 0:
                    memset('pool', ubuf[:, :, 0:30], 0.0)
                    memset('pool', hist[:], 0.0)
                    memset('pool', Hs[:], 0.0)
                nxt = order[bi + 1] if bi + 1 < len(order) else None
                run_block(l, s, tb, have_h=(bi > 0), nxt=nxt)
        S.wait_all_dma('sp')
        S.emit()
    return nc


def pack_params(inp, NL=2):
    f = lambda k: np.asarray(inp[k], dtype=np.float32)
    pp = np.zeros((NL, 128, NPP), np.float32)
    wsm = np.zeros((NL, 128, NWS), np.float32)
    fb = np.zeros((NL, 768), np.float32)
    for l in range(NL):
        pp[l, :, P_PRE:P_PRE + 8] = f('pre_norm_g')[l].reshape(8, 128).T
        pp[l, :, P_POST:P_POST + 8] = f('post_norm_g')[l].reshape(8, 128).T
        dw = f('conv_dw')[l]
        for ch in range(2):
            pp[l, :, P_DW + ch * TAPS:P_DW + (ch + 1) * TAPS] = dw[:, ch * 128:(ch + 1) * 128].T
        for key, col in (('conv_dw_b', P_DWB), ('conv_ln_g', P_LNG), ('conv_ln_b', P_LNB), ('conv_pw_b', P_PWB)):
            pp[l, :, col:col + 2] = f(key)[l].reshape(2, 128).T
        mu = f('rwkv_mu')[l]
        for hp in range(3):
            for fi in range(3):
                pp[l, :, P_MU + hp * 3 + fi] = mu[fi * 384 + hp * 128:fi * 384 + (hp + 1) * 128]
        pp[l, 0:64, P_MUWA] = mu[1152:1216]
        for key, col in (('rwkv_w0', P_W0), ('rwkv_a0', P_A0), ('rwkv_kk_scale', P_KKS), ('rwkv_ka', P_KA)):
            pp[l, :, col:col + 3] = f(key)[l].reshape(3, 128).T
        if l >= 1:
            pp[l, :, P_V0:P_V0 + 3] = f('rwkv_v0')[l - 1].reshape(3, 128).T
        pw = f('conv_pw')[l]
        for ci in range(2):
            wsm[l, :, W_PW + ci * 256:W_PW + (ci + 1) * 256] = pw[ci * 128:(ci + 1) * 128, :]
        wsm[l, 0:32, W_W2A2:W_W2A2 + 384] = f('rwkv_w2')[l]
        wsm[l, 32:64, W_W2A2:W_W2A2 + 384] = f('rwkv_a2')[l]
        if l >= 1:
            v1 = f('rwkv_v1')[l - 1]
            for hp in range(3):
                wsm[l, :, W_V1 + hp * 32:W_V1 + (hp + 1) * 32] = v1[hp * 128:(hp + 1) * 128, :]
            wsm[l, 0:32, W_V2:W_V2 + 384] = f('rwkv_v2')[l - 1]
        rk = f('rwkv_rk')[l].reshape(384)
        for hp in range(3):
            wsm[l, 0:64, W_RKM + hp * 2 + 0] = rk[hp * 128:hp * 128 + 64]
            wsm[l, 64:128, W_RKM + hp * 2 + 1] = rk[hp * 128 + 64:hp * 128 + 128]
        fb[l, 0:384] = f('rwkv_gn_g')[l]
        fb[l, 384:768] = f('rwkv_gn_b')[l]
    return pp, wsm, fb


_NC_CACHE = {}


def kernel(**inputs):
    x = np.asarray(inputs['x'], dtype=np.float32)
    B, T, _ = x.shape
    ncores = 8
    nseq = B // ncores
    pp, wsm, fb = pack_params(inputs)
    w_in = np.ascontiguousarray(np.asarray(inputs['w_in'], dtype=np.float32))
    w_out = np.ascontiguousarray(np.asarray(inputs['w_out'], dtype=np.float32))
    xT = np.ascontiguousarray(x.transpose(0, 2, 1))
    key = (T, nseq)
    if key not in _NC_CACHE:
        _NC_CACHE[key] = build(T=T, NSEQ=nseq, NL=2)
    nc = _NC_CACHE[key]
    in_maps = [{"xT": xT[c * nseq:(c + 1) * nseq], "w_in": w_in, "w_out": w_out, "pp": pp, "wsm": wsm, "fb": fb}
               for c in range(ncores)]
    res = run_bass_kernel_spmd(nc, in_maps, core_ids=list(range(ncores)))
    yT = np.concatenate([r["yT"] for r in res.results], axis=0)
    return np.ascontiguousarray(yT.transpose(0, 2, 1)).astype(np.float32)
```
